# Optimizing a Trainium2 kernel written in Bass

```python
import math
import jax, jax.numpy as jnp
from jax import lax
import numpy as np

D_MODEL = 1024
BATCH = 8
SEQ = 4096
DEPTH = 4

HEAD_DIM = 64
D_MIX = D_MODEL
NSA_WIDTH = 3 * D_MIX // 8
NSA_HEADS = NSA_WIDTH // HEAD_DIM
NSA_KV_HEADS = 2
NSA_KV_WIDTH = NSA_KV_HEADS * HEAD_DIM
CMP_BLOCK = 32
CMP_STRIDE = 16
CMP_HIDDEN = 128
SEL_BLOCK = 64
SEL_TOP = 16
WINDOW = 512
N_BRANCH = 3
MLA_WIDTH = 3 * D_MIX // 8
MLA_V = 64
MLA_HEADS = MLA_WIDTH // MLA_V
MLA_NOPE = 64
MLA_ROPE = 32
Q_LORA = 3 * D_MODEL // 8
KV_LORA = D_MODEL // 8
ROPE_THETA = 10000.0
SSM_WIDTH = D_MIX - NSA_WIDTH - MLA_WIDTH
SSM_GROUP_CH = 16
SSM_GROUPS = SSM_WIDTH // SSM_GROUP_CH
SSM_STATE = 64
DT_MIN = 1e-3
DT_MAX = 1e-1
D_FF = 2816
CONV_WIDTH = 3
Q_BLOCK = 128
SEL_Q_BLOCK = 64
EPS = 1e-6
NEG = -1e30
IN_SIZES = (NSA_WIDTH,) + (NSA_KV_WIDTH,) * 6 + (NSA_HEADS * N_BRANCH, Q_LORA, KV_LORA, MLA_ROPE, SSM_WIDTH)
D_IN = sum(IN_SIZES)

kernel_name = 'hybrid_nsa_mla_s5_convffn'


def rms_norm(x, g):
    xf = x.astype(jnp.float32)
    y = xf * lax.rsqrt(jnp.mean(xf * xf, axis=-1, keepdims=True) + EPS)
    return (y * g.astype(jnp.float32)).astype(x.dtype)


def masked_softmax(s, mask):
    p = jax.nn.softmax(jnp.where(mask, s.astype(jnp.float32), NEG), axis=-1)
    return p * mask


def rope_angles(positions):
    half = MLA_ROPE // 2
    inv_freq = ROPE_THETA ** (-jnp.arange(half, dtype=jnp.float32) / half)
    ang = positions.astype(jnp.float32)[..., None] * inv_freq
    return jnp.cos(ang), jnp.sin(ang)


def apply_rope(x, cos, sin):
    x1, x2 = jnp.split(x.astype(jnp.float32), 2, axis=-1)
    return jnp.concatenate([x1 * cos - x2 * sin, x2 * cos + x1 * sin], axis=-1).astype(x.dtype)


def compress_blocks(blocks, pe, w1, b1, w2, b2):
    h = jax.nn.gelu(jnp.einsum('bnlkd,ldf->bnkf', blocks + pe[:, None, :], w1) + b1)
    return jnp.einsum('bnkf,fd->bnkd', h, w2) + b2


def nsa_attention(q, kc, vc, ks, vs, kw, vw, gate_logits, pe,
                  ck_w1, ck_b1, ck_w2, ck_b2, cv_w1, cv_b1, cv_w2, cv_b2, gate_b):
    B, S, _ = q.shape
    G = NSA_HEADS // NSA_KV_HEADS
    q = q.reshape(B, S, NSA_KV_HEADS, G, HEAD_DIM) * (HEAD_DIM ** -0.5)
    kc, vc, ks, vs, kw, vw = [t.reshape(B, S, NSA_KV_HEADS, HEAD_DIM) for t in (kc, vc, ks, vs, kw, vw)]
    pos = jnp.arange(S)

    nc = (S - CMP_BLOCK) // CMP_STRIDE + 1
    cmp_start = jnp.arange(nc) * CMP_STRIDE
    cidx = cmp_start[:, None] + jnp.arange(CMP_BLOCK)[None, :]
    k_cmp = compress_blocks(kc[:, cidx], pe, ck_w1, ck_b1, ck_w2, ck_b2)
    v_cmp = compress_blocks(vc[:, cidx], pe, cv_w1, cv_b1, cv_w2, cv_b2)
    cmask = (cmp_start + CMP_BLOCK - 1)[None, :] <= pos[:, None]
    p_cmp = masked_softmax(jnp.einsum('btkgd,bnkd->bkgtn', q, k_cmp), cmask)
    o_cmp = jnp.einsum('bkgtn,bnkd->btkgd', p_cmp.astype(v_cmp.dtype), v_cmp)

    ns = S // SEL_BLOCK
    n_top = min(SEL_TOP, ns)
    sel_id = jnp.arange(ns)
    overlap = ((cmp_start[:, None] < (sel_id[None, :] + 1) * SEL_BLOCK)
               & (cmp_start[:, None] + CMP_BLOCK > sel_id[None, :] * SEL_BLOCK)).astype(jnp.float32)
    imp = jnp.einsum('bkgtn,nj->bktj', p_cmp, overlap)
    cur = pos // SEL_BLOCK
    forced = (sel_id[None, :] == 0) | (sel_id[None, :] == cur[:, None]) | (sel_id[None, :] == cur[:, None] - 1)
    future = sel_id[None, :] > cur[:, None]
    imp = jnp.where(forced, jnp.inf, jnp.where(future, -jnp.inf, imp))
    _, top_idx = lax.top_k(imp, n_top)

    ks_blk = ks.reshape(B, ns, SEL_BLOCK, NSA_KV_HEADS, HEAD_DIM).transpose(0, 3, 1, 2, 4)
    vs_blk = vs.reshape(B, ns, SEL_BLOCK, NSA_KV_HEADS, HEAD_DIM).transpose(0, 3, 1, 2, 4)
    nq = S // SEL_Q_BLOCK
    q_chunks = q.reshape(B, nq, SEL_Q_BLOCK, NSA_KV_HEADS, G, HEAD_DIM).transpose(1, 0, 2, 3, 4, 5)
    idx_chunks = top_idx.reshape(B, NSA_KV_HEADS, nq, SEL_Q_BLOCK, n_top).transpose(2, 0, 1, 3, 4)
    pos_chunks = pos.reshape(nq, SEL_Q_BLOCK)
    b_ix = jnp.arange(B)[:, None, None, None]
    h_ix = jnp.arange(NSA_KV_HEADS)[None, :, None, None]

    def sel_chunk(args):
        qc, ic, tc = args
        kg = ks_blk[b_ix, h_ix, ic]
        vg = vs_blk[b_ix, h_ix, ic]
        s = jnp.einsum('bqkgd,bkqnsd->bkgqns', qc, kg)
        key_pos = ic[..., None] * SEL_BLOCK + jnp.arange(SEL_BLOCK)
        m = (key_pos <= tc[None, None, :, None, None])[:, :, None]
        p = masked_softmax(s.reshape(s.shape[:4] + (-1,)), m.reshape(m.shape[:4] + (-1,)))
        return jnp.einsum('bkgqns,bkqnsd->bqkgd', p.reshape(s.shape).astype(vg.dtype), vg)

    o_sel = lax.map(sel_chunk, (q_chunks, idx_chunks, pos_chunks))
    o_sel = o_sel.transpose(1, 0, 2, 3, 4, 5).reshape(B, S, NSA_KV_HEADS, G, HEAD_DIM)

    nqb = S // Q_BLOCK
    span = WINDOW + Q_BLOCK
    pad = ((0, 0), (WINDOW, 0), (0, 0), (0, 0))
    widx = jnp.arange(nqb)[:, None] * Q_BLOCK + jnp.arange(span)[None, :]
    kwb = jnp.pad(kw, pad)[:, widx]
    vwb = jnp.pad(vw, pad)[:, widx]
    qb = q.reshape(B, nqb, Q_BLOCK, NSA_KV_HEADS, G, HEAD_DIM)
    s_win = jnp.einsum('biqkgd,bijkd->bikgqj', qb, kwb)
    q_pos = pos.reshape(nqb, Q_BLOCK)[:, :, None]
    k_pos = (widx - WINDOW)[:, None, :]
    wmask = (k_pos <= q_pos) & (k_pos > q_pos - WINDOW) & (k_pos >= 0)
    p_win = masked_softmax(s_win, wmask[None, :, None, None])
    o_win = jnp.einsum('bikgqj,bijkd->biqkgd', p_win.astype(vwb.dtype), vwb).reshape(B, S, NSA_KV_HEADS, G, HEAD_DIM)

    g = jax.nn.sigmoid((gate_logits + gate_b).astype(jnp.float32)).reshape(B, S, NSA_KV_HEADS, G, N_BRANCH).astype(q.dtype)
    o = g[..., 0:1] * o_cmp + g[..., 1:2] * o_sel + g[..., 2:3] * o_win
    return o.reshape(B, S, NSA_WIDTH)


def mla_attention(c_q, c_kv, k_rope, cos, sin, q_norm, kv_norm, w_uq, w_uk, w_uv):
    B, S, _ = c_q.shape
    H = MLA_HEADS
    q = (rms_norm(c_q, q_norm) @ w_uq).reshape(B, S, H, MLA_NOPE + MLA_ROPE)
    ckv = rms_norm(c_kv, kv_norm)
    k_nope = (ckv @ w_uk).reshape(B, S, H, MLA_NOPE)
    v = (ckv @ w_uv).reshape(B, S, H, MLA_V)
    scale = (MLA_NOPE + MLA_ROPE) ** -0.5
    q_nope = q[..., :MLA_NOPE] * scale
    q_rope = apply_rope(q[..., MLA_NOPE:], cos[:, :, None], sin[:, :, None]) * scale
    k_rope = apply_rope(k_rope, cos, sin)
    nqb = S // Q_BLOCK
    qn_b = q_nope.reshape(B, nqb, Q_BLOCK, H, MLA_NOPE).transpose(1, 0, 2, 3, 4)
    qr_b = q_rope.reshape(B, nqb, Q_BLOCK, H, MLA_ROPE).transpose(1, 0, 2, 3, 4)
    q_pos = jnp.arange(S).reshape(nqb, Q_BLOCK)
    k_pos = jnp.arange(S)

    def block(args):
        qn, qr, qp = args
        s = jnp.einsum('bqhd,bshd->bhqs', qn, k_nope) + jnp.einsum('bqhr,bsr->bhqs', qr, k_rope)
        p = masked_softmax(s, k_pos[None, :] <= qp[:, None])
        return jnp.einsum('bhqs,bshd->bqhd', p.astype(v.dtype), v)

    o = lax.map(block, (qn_b, qr_b, q_pos))
    return o.transpose(1, 0, 2, 3, 4).reshape(B, S, MLA_WIDTH)


def ssm_combine(e1, e2):
    a1, b1 = e1
    a2, b2 = e2
    return a1 * a2, a2 * b1 + b2


def s5_ssm(u, log_dt, a_re, a_im, b_re, b_im, c_re, c_im, d, w_glu, b_glu):
    B, S, _ = u.shape
    f32 = jnp.float32
    uf = u.astype(f32).reshape(B, S, SSM_GROUPS, SSM_GROUP_CH)
    a = lax.complex(a_re.astype(f32), a_im.astype(f32))
    dt = jnp.exp(log_dt.astype(f32))[:, None]
    a_bar = jnp.exp(a * dt)
    b_bar = ((a_bar - 1.0) / a)[..., None] * lax.complex(b_re.astype(f32), b_im.astype(f32))
    bu = jnp.einsum('bsgc,gpc->bsgp', uf.astype(jnp.complex64), b_bar)
    _, h = lax.associative_scan(ssm_combine, (jnp.broadcast_to(a_bar, bu.shape), bu), axis=1)
    c = lax.complex(c_re.astype(f32), c_im.astype(f32))
    y = jnp.einsum('bsgp,gcp->bsgc', h, c).real + d.astype(f32) * uf
    z = jax.nn.gelu(y.reshape(B, S, SSM_WIDTH).astype(u.dtype))
    return z * jax.nn.sigmoid(z @ w_glu + b_glu)


def conv_ffn(h, w_up, conv_w, conv_b, w_down):
    gate, val = jnp.split(h @ w_up, 2, axis=-1)
    S = gate.shape[1]
    gp = jnp.pad(gate, ((0, 0), (CONV_WIDTH - 1, 0), (0, 0)))
    gate = sum(gp[:, k:k + S] * conv_w[k] for k in range(CONV_WIDTH)) + conv_b
    return (jax.nn.silu(gate) * val) @ w_down


def setup_inputs(seed: int = 0) -> dict:
    key = jax.random.key(seed)
    ks = iter(jax.random.split(key, 48))
    L = DEPTH
    f32 = jnp.float32

    def normal(shape, scale):
        return jax.random.normal(next(ks), shape, f32) * scale

    def gain(shape):
        return 1.0 + normal(shape, 0.02)

    x = jax.random.normal(next(ks), (BATCH, SEQ, D_MODEL), f32)
    positions = jax.random.randint(next(ks), (BATCH, 1), 0, 1024, dtype=jnp.int32) + jnp.arange(SEQ, dtype=jnp.int32)[None, :]
    return {
        'x': x,
        'positions': positions,
        'attn_norm': gain((L, D_MODEL)),
        'w_in': normal((L, D_MODEL, D_IN), D_MODEL ** -0.5),
        'nsa_pe': normal((L, CMP_BLOCK, HEAD_DIM), 0.1),
        'nsa_ck_w1': normal((L, CMP_BLOCK, HEAD_DIM, CMP_HIDDEN), (CMP_BLOCK * HEAD_DIM) ** -0.5),
        'nsa_ck_b1': normal((L, CMP_HIDDEN), 0.01),
        'nsa_ck_w2': normal((L, CMP_HIDDEN, HEAD_DIM), CMP_HIDDEN ** -0.5),
        'nsa_ck_b2': normal((L, HEAD_DIM), 0.01),
        'nsa_cv_w1': normal((L, CMP_BLOCK, HEAD_DIM, CMP_HIDDEN), (CMP_BLOCK * HEAD_DIM) ** -0.5),
        'nsa_cv_b1': normal((L, CMP_HIDDEN), 0.01),
        'nsa_cv_w2': normal((L, CMP_HIDDEN, HEAD_DIM), CMP_HIDDEN ** -0.5),
        'nsa_cv_b2': normal((L, HEAD_DIM), 0.01),
        'nsa_gate_b': normal((L, NSA_HEADS * N_BRANCH), 0.1),
        'mla_q_norm': gain((L, Q_LORA)),
        'mla_kv_norm': gain((L, KV_LORA)),
        'mla_w_uq': normal((L, Q_LORA, MLA_HEADS * (MLA_NOPE + MLA_ROPE)), Q_LORA ** -0.5),
        'mla_w_uk': normal((L, KV_LORA, MLA_HEADS * MLA_NOPE), KV_LORA ** -0.5),
        'mla_w_uv': normal((L, KV_LORA, MLA_HEADS * MLA_V), KV_LORA ** -0.5),
        'ssm_log_dt': jax.random.uniform(next(ks), (L, SSM_GROUPS), f32, math.log(DT_MIN), math.log(DT_MAX)),
        'ssm_a_re': -0.5 + normal((L, SSM_GROUPS, SSM_STATE), 0.01),
        'ssm_a_im': math.pi * jnp.arange(SSM_STATE, dtype=f32) + normal((L, SSM_GROUPS, SSM_STATE), 0.01),
        'ssm_b_re': normal((L, SSM_GROUPS, SSM_STATE, SSM_GROUP_CH), (2 * SSM_GROUP_CH) ** -0.5),
        'ssm_b_im': normal((L, SSM_GROUPS, SSM_STATE, SSM_GROUP_CH), (2 * SSM_GROUP_CH) ** -0.5),
        'ssm_c_re': normal((L, SSM_GROUPS, SSM_GROUP_CH, SSM_STATE), SSM_STATE ** -0.5),
        'ssm_c_im': normal((L, SSM_GROUPS, SSM_GROUP_CH, SSM_STATE), SSM_STATE ** -0.5),
        'ssm_d': normal((L, SSM_GROUPS, SSM_GROUP_CH), 0.5),
        'ssm_w_glu': normal((L, SSM_WIDTH, SSM_WIDTH), SSM_WIDTH ** -0.5),
        'ssm_b_glu': normal((L, SSM_WIDTH), 0.01),
        'out_norm_nsa': gain((L, NSA_WIDTH)),
        'out_norm_mla': gain((L, MLA_WIDTH)),
        'out_norm_ssm': gain((L, SSM_WIDTH)),
        'w_out': normal((L, D_MIX, D_MODEL), D_MIX ** -0.5),
        'ffn_norm': gain((L, D_MODEL)),
        'ffn_w_up': normal((L, D_MODEL, 2 * D_FF), D_MODEL ** -0.5),
        'ffn_conv_w': normal((L, CONV_WIDTH, D_FF), CONV_WIDTH ** -0.5),
        'ffn_conv_b': normal((L, D_FF), 0.01),
        'ffn_w_down': normal((L, D_FF, D_MODEL), D_FF ** -0.5),
        'final_norm': gain((D_MODEL,)),
    }


def reference(x, positions, attn_norm, w_in, nsa_pe,
              nsa_ck_w1, nsa_ck_b1, nsa_ck_w2, nsa_ck_b2,
              nsa_cv_w1, nsa_cv_b1, nsa_cv_w2, nsa_cv_b2, nsa_gate_b,
              mla_q_norm, mla_kv_norm, mla_w_uq, mla_w_uk, mla_w_uv,
              ssm_log_dt, ssm_a_re, ssm_a_im, ssm_b_re, ssm_b_im, ssm_c_re, ssm_c_im, ssm_d,
              ssm_w_glu, ssm_b_glu, out_norm_nsa, out_norm_mla, out_norm_ssm, w_out,
              ffn_norm, ffn_w_up, ffn_conv_w, ffn_conv_b, ffn_w_down, final_norm):
    split_points = np.cumsum(IN_SIZES)[:-1]
    cos, sin = rope_angles(positions)
    for l in range(DEPTH):
        h = rms_norm(x, attn_norm[l])
        (q_a, k_c, v_c, k_s, v_s, k_w, v_w, gate_a,
         c_q, c_kv, k_r, u) = jnp.split(h @ w_in[l], split_points, axis=-1)
        o_a = nsa_attention(q_a, k_c, v_c, k_s, v_s, k_w, v_w, gate_a, nsa_pe[l],
                            nsa_ck_w1[l], nsa_ck_b1[l], nsa_ck_w2[l], nsa_ck_b2[l],
                            nsa_cv_w1[l], nsa_cv_b1[l], nsa_cv_w2[l], nsa_cv_b2[l], nsa_gate_b[l])
        o_b = mla_attention(c_q, c_kv, k_r, cos, sin, mla_q_norm[l], mla_kv_norm[l],
                            mla_w_uq[l], mla_w_uk[l], mla_w_uv[l])
        o_c = s5_ssm(u, ssm_log_dt[l], ssm_a_re[l], ssm_a_im[l], ssm_b_re[l], ssm_b_im[l],
                     ssm_c_re[l], ssm_c_im[l], ssm_d[l], ssm_w_glu[l], ssm_b_glu[l])
        mix = jnp.concatenate([rms_norm(o_a, out_norm_nsa[l]),
                               rms_norm(o_b, out_norm_mla[l]),
                               rms_norm(o_c, out_norm_ssm[l])], axis=-1)
        x = x + mix @ w_out[l]
        x = x + conv_ffn(rms_norm(x, ffn_norm[l]), ffn_w_up[l], ffn_conv_w[l], ffn_conv_b[l], ffn_w_down[l])
    return rms_norm(x, final_norm)
```

```python
import contextlib
import math
import numpy as np
import concourse.bass as bass
import concourse.mybir as mybir
from concourse.bass_utils import run_bass_kernel_spmd

F32 = mybir.dt.float32
BF16 = mybir.dt.bfloat16
I32 = mybir.dt.int32
ALU = mybir.AluOpType
AF = mybir.ActivationFunctionType
AX = mybir.AxisListType

SAME_ENG_SYNC = True
SEM_ROLL = 8000
NDMA = 8

S = 4096
D = 1024
NT = 32
DEPTH = 4
DIN = 1970
DFF = 2816
EPS = 1e-6
NEG = -30000.0
TWO_PI = 2.0 * math.pi
SAFE_2PI = 6.28318


class Trk:
    __slots__ = ("name", "w", "r")

    def __init__(self, name=""):
        self.name = name
        self.w = None
        self.r = {}


class T:
    def __init__(self, h, name, n=0):
        self.h = h
        self.t = Trk(name)
        self.ts = [Trk(f"{name}{i}") for i in range(n)]

    def __getitem__(self, k):
        return self.h[k]


class Op:
    __slots__ = ("eng", "fn", "deps", "sig", "dma", "event", "pseudo")

    def __init__(self, eng, fn, dma):
        self.eng = eng
        self.fn = fn
        self.deps = set()
        self.sig = False
        self.dma = dma
        self.event = None
        self.pseudo = fn is None


class KB:
    def __init__(self, nc):
        self.nc = nc
        self.es = contextlib.ExitStack()
        self.eng = {"pe": nc.tensor, "dve": nc.vector, "act": nc.scalar, "pool": nc.gpsimd, "sp": nc.sync}
        self.ops = []
        self.nsem = 0
        self.nname = 0

    def sbuf(self, name, shape, dtype, n=0, es=None):
        self.nname += 1
        h = (es or self.es).enter_context(self.nc.sbuf_tensor(f"{name}_{self.nname}", list(shape), dtype))
        return T(h, name, n)

    def psum(self, name, shape, dtype, es=None):
        self.nname += 1
        h = (es or self.es).enter_context(self.nc.psum_tensor(f"{name}_{self.nname}", list(shape), dtype))
        return T(h, name)

    def dram(self, name, shape, dtype, kind="Internal", n=0):
        h = self.nc.dram_tensor(name, list(shape), dtype, kind=kind)
        return T(h.ap(), name, n)

    def newsem(self):
        self.nsem += 1
        return self.es.enter_context(self.nc.semaphore(f"s{self.nsem}"))

    def _rec(self, eng, fn, reads, writes, dma):
        idx = len(self.ops)
        o = Op(eng, fn, dma)
        deps = set()
        for t in reads:
            if t.w is not None:
                deps.add(t.w)
        for t in writes:
            if t.w is not None:
                deps.add(t.w)
            deps.update(t.r.values())
        for d in deps:
            od = self.ops[d]
            if (not od.dma) and od.eng == eng and not dma:
                if eng == "pe" or not SAME_ENG_SYNC:
                    continue
            o.deps.add(d)
            od.sig = True
        for t in reads:
            if dma:
                t.r[("dma", idx)] = idx
            else:
                t.r[eng] = idx
        for t in writes:
            t.w = idx
            t.r = {}
        self.ops.append(o)
        return idx

    def op(self, eng, fn, reads=(), writes=()):
        return self._rec(eng, fn, reads, writes, False)

    def dma(self, out, in_, reads=(), writes=(), eng="sp", **kw):
        return self._rec(eng, lambda e: e.dma_start(out=out, in_=in_, **kw), reads, writes, True)

    def barrier(self):
        last = {}
        start = getattr(self, "_bar_start", 0)
        for i, o in enumerate(self.ops):
            if o.pseudo:
                continue
            if o.dma:
                if i >= start:
                    last[("dma", i)] = i
            else:
                last[o.eng] = i
        deps = set(last.values())
        self._bar_start = len(self.ops)
        for e in ("pe", "dve", "act", "pool", "sp"):
            o = Op(e, None, False)
            for d in deps:
                od = self.ops[d]
                if (not od.dma) and od.eng == e and e == "pe":
                    continue
                o.deps.add(d)
                od.sig = True
            self.ops.append(o)

    def _init_emit(self):
        self.sem = {}
        self.cnt = {}
        self.epoch = {}
        self.known = {e: {} for e in self.eng}
        self.dma_slots = {}
        self.dma_i = {}
        self.final_events = {}
        self.emitted = 0
        self._einit = True

    def cur_sem(self, e):
        if e not in self.sem or self.cnt[e] >= SEM_ROLL:
            self.epoch[e] = self.epoch.get(e, -1) + 1
            self.sem[e] = (self.newsem(), (e, self.epoch[e]))
            self.cnt[e] = 0
        return self.sem[e]

    def flush(self, final=False):
        if not getattr(self, "_einit", False):
            self._init_emit()
        nc = self.nc
        known = self.known
        batch = self.ops[self.emitted:]
        lastop = {}
        for o in batch:
            if (not o.dma) and not o.pseudo:
                lastop[o.eng] = o
        for o in lastop.values():
            o.sig = True
        for o in batch:
            E = self.eng[o.eng]
            waits = {}
            for d in o.deps:
                ev = self.ops[d].event
                assert ev is not None, "dep without event"
                h, key, val = ev
                if known[o.eng].get(key, 0) >= val:
                    continue
                if key not in waits or waits[key][1] < val:
                    waits[key] = (h, val)
            slot = None
            if o.dma:
                sl = self.dma_slots.setdefault(o.eng, [None] * NDMA)
                i = self.dma_i.get(o.eng, 0)
                self.dma_i[o.eng] = i + 1
                s = sl[i % NDMA]
                if s is None or s[2] >= SEM_ROLL:
                    if s is not None and known[o.eng].get(s[1], 0) < s[2]:
                        waits[s[1]] = (s[0], s[2])
                    s = [self.newsem(), ("dma", o.eng, i % NDMA, self.nsem), 0]
                    sl[i % NDMA] = s
                elif s[2] > 0 and known[o.eng].get(s[1], 0) < s[2]:
                    if s[1] not in waits or waits[s[1]][1] < s[2]:
                        waits[s[1]] = (s[0], s[2])
                slot = s
            for key, (h, val) in waits.items():
                E.wait_ge(h, val)
                known[o.eng][key] = val
            if o.pseudo:
                continue
            ins = o.fn(E)
            if o.dma:
                slot[2] += 16
                ins.then_inc(slot[0], 16)
                o.event = (slot[0], slot[1], slot[2])
                self.final_events[slot[1]] = (slot[0], slot[2])
            elif o.sig:
                h, key = self.cur_sem(o.eng)
                self.cnt[o.eng] += 1
                ins.then_inc(h, 1)
                o.event = (h, key, self.cnt[o.eng])
        nxt = {}
        for o in reversed(batch):
            if o.dma or o.pseudo:
                continue
            if o.event is None:
                o.event = nxt[o.eng]
            else:
                nxt[o.eng] = o.event
            o.fn = None
        self.emitted = len(self.ops)
        if final:
            for key, (h, val) in self.final_events.items():
                if known["sp"].get(key, 0) < val:
                    nc.sync.wait_ge(h, val)
                    known["sp"][key] = val

    def close(self):
        self.es.close()


def act(kb, out, in_, func, reads, writes, eng="act", **kw):
    kb.op(eng, lambda e: e.activation(out=out, in_=in_, func=func, **kw), reads, writes)


def tcopy(kb, eng, out, in_, reads, writes):
    if eng == "act":
        kb.op("act", lambda e: e.activation(out=out, in_=in_, func=AF.Copy), reads, writes)
    else:
        kb.op(eng, lambda e: e.tensor_copy(out=out, in_=in_), reads, writes)


def tt(kb, eng, out, in0, in1, op, reads, writes):
    kb.op(eng, lambda e: e.tensor_tensor(out=out, in0=in0, in1=in1, op=op), reads, writes)


def ts(kb, eng, out, in0, s1, s2, op0, op1, reads, writes, **kw):
    if op1 is None:
        kb.op(eng, lambda e: e.tensor_scalar(out=out, in0=in0, scalar1=s1, scalar2=None, op0=op0, **kw), reads, writes)
    else:
        kb.op(eng, lambda e: e.tensor_scalar(out=out, in0=in0, scalar1=s1, scalar2=s2, op0=op0, op1=op1, **kw), reads, writes)


def stt(kb, out, in0, scalar, in1, op0, op1, reads, writes):
    kb.op("dve", lambda e: e.scalar_tensor_tensor(out=out, in0=in0, scalar=scalar, in1=in1, op0=op0, op1=op1), reads, writes)


def mm(kb, out, lhsT, rhs, start, reads, writes):
    kb.op("pe", lambda e: e.matmul(out, lhsT=lhsT, rhs=rhs, start=start, stop=True, skip_group_check=True), reads, writes)


def tr(kb, out, in_, ident, reads, writes):
    kb.op("pe", lambda e: e.transpose(out=out, in_=in_, identity=ident), reads, writes)


def memset(kb, eng, ap, val, writes):
    kb.op(eng, lambda e: e.memset(ap, val), [], writes)


def asel(kb, out, in_, pattern, op, fill, base, cm, reads, writes):
    kb.op("pool", lambda e: e.affine_select(out=out, in_=in_, pattern=pattern, compare_op=op, fill=fill, base=base,
                                            channel_multiplier=cm), reads, writes)


def bcast(ap, dims):
    lst = [list(x) for x in ap.ap]
    for pos, cnt in dims:
        lst.insert(pos, [0, cnt])
    return bass.AP(ap.tensor, ap.offset, lst)


class Ctx:
    pass


def make_stage(kb, C, es, cols=2048):
    C.stage = [kb.sbuf(f"stage{i}", [128, cols], F32, es=es) for i in range(2)]


def rstd_of(kb, C, src, n, reads, key):
    i = C.rs_i = getattr(C, "rs_i", 0) + 1
    ssq = C.rs_ssq[i % 4]
    rs = C.rs_out[i % 4]
    junk = C.rs_junk
    act(kb, junk[:, 0:n], src, AF.Square, reads, [junk.t, ssq.t] + ([key] if key is not None else []), accum_out=ssq[:, 0:1])
    ts(kb, "dve", ssq[:, 1:2], ssq[:, 0:1], 1.0 / n, EPS, ALU.mult, ALU.add, [ssq.t], [ssq.t])
    act(kb, ssq[:, 2:3], ssq[:, 1:2], AF.Sqrt, [ssq.t], [ssq.t])
    kb.op("dve", lambda e: e.reciprocal(out=rs[:, 0:1], in_=ssq[:, 2:3]), [ssq.t], [rs.t])
    return rs


def gelu_tanh(kb, out, x, tmp1, tmp2, reads, writes, tmp_trk):
    c = math.sqrt(2.0 / math.pi)
    tt(kb, "dve", tmp1, x, x, ALU.mult, reads, [tmp_trk])
    ts(kb, "dve", tmp1, tmp1, 0.044715, 1.0, ALU.mult, ALU.add, [tmp_trk], [tmp_trk])
    tt(kb, "dve", tmp1, tmp1, x, ALU.mult, reads + [tmp_trk], [tmp_trk])
    act(kb, tmp2, tmp1, AF.Tanh, [tmp_trk], [tmp_trk], scale=c)
    ts(kb, "dve", tmp2, tmp2, 0.5, 0.5, ALU.mult, ALU.add, [tmp_trk], [tmp_trk])
    tt(kb, "dve", out, tmp2, x, ALU.mult, reads + [tmp_trk], writes)


def load_cols(kb, C, dst_ap, dst_trk, src_flat, ncol, bank):
    i = C.lc_i = getattr(C, "lc_i", 0) + 1
    st = C.lc_stage[i % 2]
    npad = 8 if ncol <= 8 else 32
    kb.dma(st[0:ncol, :], src_flat.rearrange("(c p) -> c p", p=128), writes=[st.t])
    tr(kb, bank[:, 0:npad], st[0:npad, :], C.identf[0:npad, 0:npad], [st.t, C.identf.t], [bank.t])
    tmp = C.lc_tmp[i % 2]
    tcopy(kb, "dve", tmp[:, 0:npad], bank[:, 0:npad], [], [bank.t, tmp.t])
    tcopy(kb, "dve", dst_ap, tmp[:, 0:ncol], [tmp.t], [dst_trk])


def stage_load(kb, C, src_ap, rows, cols):
    i = C.st_i = getattr(C, "st_i", 0) + 1
    st = C.stage[i % len(C.stage)]
    kb.dma(st[0:rows, 0:cols], src_ap, writes=[st.t])
    return st


def cast_scaled(kb, C, out, in_, scale_ap, reads, writes):
    i = C.cs_i = getattr(C, "cs_i", 0) + 1
    if scale_ap is None:
        if i % 2:
            tcopy(kb, "pool", out, in_, reads, writes)
        else:
            tcopy(kb, "act", out, in_, reads, writes)
    else:
        if i % 2:
            ts(kb, "pool", out, in_, scale_ap, 0.0, ALU.mult, ALU.add, reads, writes)
        else:
            act(kb, out, in_, AF.Copy, reads, writes, scale=scale_ap)


def setup_consts(kb, C):
    C.ident = kb.sbuf("ident", [128, 128], BF16)
    C.identf = kb.sbuf("identf", [128, 128], F32)
    for t in (C.ident, C.identf):
        memset(kb, "pool", t[:], 0.0, [t.t])
        asel(kb, t[:], t[:], [[-1, 128]], ALU.not_equal, 1.0, 0, 1, [t.t], [t.t])
    C.ones = kb.sbuf("ones", [128, 128], BF16)
    memset(kb, "pool", C.ones[:], 1.0, [C.ones.t])
    C.Ov = kb.sbuf("Ov", [128, 2, 64], BF16)
    memset(kb, "pool", C.Ov[:], 1.0, [C.Ov.t])
    for kt in range(2):
        asel(kb, C.Ov[:, kt, :], C.Ov[:, kt, :], [[-4, 64]], ALU.is_ge, 0.0, 128 * kt + 1, 1, [C.Ov.t], [C.Ov.t])
        asel(kb, C.Ov[:, kt, :], C.Ov[:, kt, :], [[4, 64]], ALU.is_ge, 0.0, 3 - 128 * kt, -1, [C.Ov.t], [C.Ov.t])
    C.rs_ssq = [kb.sbuf(f"rsq{i}", [128, 4], F32) for i in range(4)]
    C.rs_out = [kb.sbuf(f"rso{i}", [128, 1], F32) for i in range(4)]
    C.rs_junk = kb.sbuf("rsjunk", [128, 1024], F32)
    C.lc_stage = [kb.sbuf(f"lcs{i}", [32, 128], F32) for i in range(2)]
    C.lc_tmp = [kb.sbuf(f"lct{i}", [128, 32], F32) for i in range(2)]
    for t in C.lc_stage:
        memset(kb, "pool", t[:], 0.0, [t.t])
    C.invf = kb.sbuf("invf", [96, 1], F32)
    kb.dma(C.invf[:], C.invf_d[:], writes=[C.invf.t])


def sincos_from_turns(kb, x_ap, sin_ap, cos_ap, tmpi_ap, trk, shape_reads):
    tcopy(kb, "dve", tmpi_ap, x_ap, shape_reads + [trk], [trk])
    tt(kb, "dve", x_ap, x_ap, tmpi_ap, ALU.subtract, [trk], [trk])
    act(kb, sin_ap, x_ap, AF.Sin, [trk], [trk], scale=SAFE_2PI)
    act(kb, x_ap, x_ap, AF.Abs, [trk], [trk])
    act(kb, cos_ap, x_ap, AF.Sin, [trk], [trk], scale=-SAFE_2PI, bias=C_HALF_PI[0][0:x_ap.shape[0], 0:1])


C_HALF_PI = [None]


def phase1(kb, C, l, LS):
    P = C.P
    with contextlib.ExitStack() as es:
        make_stage(kb, C, es)
        Win = kb.sbuf("Win", [128, 8, DIN], BF16, es=es)
        Wtok = kb.sbuf("Wtok", [128, 8, 786], BF16, es=es)
        gcol = kb.sbuf("gcol", [128, 8], F32, es=es)
        TR = kb.psum("p1TR", [128, 1024], BF16, es=es)
        TR2 = kb.psum("p1TR2", [128, 1024], BF16, es=es)
        tkA = kb.psum("p1tkA", [128, 512], F32, es=es)
        tkB = kb.psum("p1tkB", [128, 512], F32, es=es)
        FM = [kb.psum(f"p1FM{i}", [128, 512], F32, es=es) for i in range(3)]
        load_cols(kb, C, gcol[:], gcol.t, P["attn_norm"][l], 8, FM[0])
        gb = kb.sbuf("gateb", [128, 18], F32, es=es)
        kb.dma(gb[:], bass.AP(P["nsa_gate_b"].h.tensor, l * 18, [[0, 128], [1, 18]]), writes=[gb.t])
        for kc in range(8):
            st = stage_load(kb, C, P["w_in"][l, kc * 128:(kc + 1) * 128, :], 128, DIN)
            sc = gcol[:, kc:kc + 1]
            cast_scaled(kb, C, Win[:, kc, 0:384].rearrange("p (b a c) -> p a b c", b=3, a=2),
                        st[:, 0:384].rearrange("p (a b c) -> p a b c", a=2, b=3), sc, [st.t, gcol.t], [Win.t])
            cast_scaled(kb, C, Win[:, kc, 384:DIN], st[:, 384:DIN], sc, [st.t, gcol.t], [Win.t])
            cast_scaled(kb, C, Wtok[:, kc, 0:512], st[:, 1170:1682], sc, [st.t, gcol.t], [Wtok.t])
            cast_scaled(kb, C, Wtok[:, kc, 512:640], st[:, 768:896], sc, [st.t, gcol.t], [Wtok.t])
            cast_scaled(kb, C, Wtok[:, kc, 640:786], st[:, 1024:1170], sc, [st.t, gcol.t], [Wtok.t])
        hTs = [kb.sbuf(f"hT{i}", [128, 8, 512], BF16, es=es) for i in range(2)]
        xbs = [kb.sbuf(f"xb{i}", [128, D], F32, es=es) for i in range(2)]
        xns = [kb.sbuf(f"xn{i}", [128, D], BF16, es=es) for i in range(2)]
        lats = [kb.sbuf(f"lat{i}", [128, 512], BF16, es=es) for i in range(2)]
        latTs = [kb.sbuf(f"latT{i}", [128, 4, 128], BF16, es=es) for i in range(2)]
        gtmp = kb.sbuf("gtmp", [128, 18], F32, es=es)
        ust = [kb.sbuf(f"ust{i}", [128, 2, 512], BF16, es=es) for i in range(1)]
        krs = [kb.sbuf(f"krs{i}", [16, 2, 512], F32, es=es) for i in range(1)]
        xin = C.x_d if l == 0 else C.XR
        fmi = 0
        for tg in range(8):
            hT = hTs[tg % 2]
            for tl in range(4):
                tt_ = tg * 4 + tl
                xt = xbs[tt_ % 2]
                xn = xns[tt_ % 2]
                kb.dma(xt[:], xin[tt_ * 128:(tt_ + 1) * 128, :], reads=[xin.t], writes=[xt.t])
                rs = rstd_of(kb, C, xt[:], D, [xt.t], None)
                ts(kb, "dve", xn[:], xt[:], rs[:, 0:1], None, ALU.mult, None, [xt.t, rs.t], [xn.t])
                for c in range(8):
                    tr(kb, TR[:, c * 128:(c + 1) * 128], xn[:, c * 128:(c + 1) * 128], C.ident[:], [xn.t, C.ident.t], [TR.t])
                tcopy(kb, "act", hT[:, :, tl * 128:(tl + 1) * 128], TR[:].rearrange("p (c t) -> p c t", c=8), [], [TR.t, hT.t])
                for kc in range(8):
                    mm(kb, tkA[:, 0:512], hT[:, kc, tl * 128:(tl + 1) * 128], Wtok[:, kc, 0:512], kc == 0, [hT.t, Wtok.t], [tkA.t])
                for kc in range(8):
                    mm(kb, tkB[:, 0:274], hT[:, kc, tl * 128:(tl + 1) * 128], Wtok[:, kc, 512:786], kc == 0, [hT.t, Wtok.t], [tkB.t])
                tcopy(kb, "act", LS.Vs[:, tt_, :, 0:64], tkB[:, 0:128].rearrange("p (k d) -> p k d", k=2), [], [tkB.t, LS.Vs.t])
                tcopy(kb, "dve", LS.Vw[:, tt_, :, 0:64], tkB[:, 128:256].rearrange("p (k d) -> p k d", k=2), [], [tkB.t, LS.Vw.t])
                tt(kb, "dve", gtmp[:], tkB[:, 256:274], gb[:], ALU.add, [gb.t], [tkB.t, gtmp.t])
                act(kb, LS.Gt[:, tt_, :], gtmp[:], AF.Sigmoid, [gtmp.t], [LS.Gt.t])
                lat = lats[tt_ % 2]
                latT = latTs[tt_ % 2]
                rq = rstd_of(kb, C, tkA[:, 0:384], 384, [], tkA.t)
                ts(kb, "dve", lat[:, 0:384], tkA[:, 0:384], rq[:, 0:1], None, ALU.mult, None, [rq.t], [tkA.t, lat.t])
                rk = rstd_of(kb, C, tkA[:, 384:512], 128, [], tkA.t)
                ts(kb, "dve", lat[:, 384:512], tkA[:, 384:512], rk[:, 0:1], None, ALU.mult, None, [rk.t], [tkA.t, lat.t])
                for c in range(4):
                    tr(kb, TR2[:, c * 128:(c + 1) * 128], lat[:, c * 128:(c + 1) * 128], C.ident[:], [lat.t, C.ident.t], [TR2.t])
                tcopy(kb, "act", latT[:], TR2[:, 0:512].rearrange("p (c t) -> p c t", c=4), [], [TR2.t, latT.t])
                kb.dma(C.LATT[:, tt_ * 128:(tt_ + 1) * 128].rearrange("(c p) t -> p c t", p=128), latT[:], reads=[latT.t], writes=[C.LATT.t])
            cols = slice(tg * 512, (tg + 1) * 512)
            specs = [(0, 128, LS.QaT[:, 0, cols], LS.QaT.t), (128, 128, LS.QaT[:, 1, cols], LS.QaT.t), (256, 128, LS.QaT[:, 2, cols], LS.QaT.t),
                     (384, 128, LS.KcT[:, cols], LS.KcT.t), (512, 128, LS.VcT[:, cols], LS.VcT.t),
                     (640, 128, LS.KsT, LS.KsT.t), (896, 128, LS.KwT, LS.KwT.t)]
            u_st = ust[0]
            kr_st = krs[0]
            specs += [(1714, 128, u_st[:, 0, :], u_st.t), (1842, 128, u_st[:, 1, :], u_st.t),
                      (1682, 16, kr_st[:, 0, :], kr_st.t), (1698, 16, kr_st[:, 1, :], kr_st.t)]
            for (c0, w, dst, dtrk) in specs:
                bank = FM[fmi % 3]
                fmi += 1
                for kc in range(8):
                    mm(kb, bank[0:w, :], Win[:, kc, c0:c0 + w], hT[:, kc, :], kc == 0, [Win.t, hT.t], [bank.t])
                if dst is LS.KsT or dst is LS.KwT:
                    tcopy(kb, "act", dst[0:64, 0, cols], bank[0:64, :], [], [bank.t, dtrk])
                    tcopy(kb, "dve", dst[64:128, 1, cols], bank[64:128, :], [], [bank.t, dtrk])
                else:
                    tcopy(kb, "act" if fmi % 2 else "dve", dst, bank[0:w, :], [], [bank.t, dtrk])
            kb.dma(C.UT[:, cols].rearrange("(c p) t -> p c t", p=128), u_st[:], reads=[u_st.t], writes=[C.UT.t])
            kb.dma(C.KR12[:, cols].rearrange("(two r) t -> r two t", two=2), kr_st[:], reads=[kr_st.t], writes=[C.KR12.t])
        kb.barrier()
        kb.flush()


def nsa_compress(kb, C, l, LS, es):
    import os
    steps = os.environ.get("CMP_STEPS", "init,pe,w,dma4,gath,mm1,mmpe,gelu,mm2k,mm2v").split(",")
    P = C.P
    KcmpT = kb.sbuf("KcmpT", [128, 2, 256], BF16, es=es)
    Vcmp = kb.sbuf("Vcmp", [128, 2, 2, 66], BF16, es=es)
    if "init" in steps:
        memset(kb, "pool", KcmpT[:], 0.0, [KcmpT.t])
        memset(kb, "pool", Vcmp[:], 0.0, [Vcmp.t])
        memset(kb, "pool", Vcmp[:, :, :, 64:65], 1.0, [Vcmp.t])
    with contextlib.ExitStack() as es2:
        make_stage(kb, C, es2, 1024)
        W1 = kb.sbuf("W1", [128, 32, 128], BF16, es=es2)
        W2 = kb.sbuf("W2", [128, 128], BF16, es=es2)
        peT = kb.sbuf("peT", [128, 32], BF16, es=es2)
        gath = kb.sbuf("gath", [128, 32, 256], BF16, es=es2)
        b2bc = kb.sbuf("b2bc", [128, 64], F32, es=es2)
        pe_st = kb.sbuf("pe_st", [32, 64], F32, es=es2)
        pe_b = kb.sbuf("pe_b", [32, 128], BF16, es=es2)
        hb = kb.sbuf("hb", [128, 2], F32, es=es2)
        b2c = kb.sbuf("b2c", [128, 1], F32, es=es2)
        hx = kb.sbuf("hx", [128, 256], F32, es=es2)
        ht1 = kb.sbuf("ht1", [128, 256], F32, es=es2)
        ht2 = kb.sbuf("ht2", [128, 256], F32, es=es2)
        hidT = kb.sbuf("hidT", [128, 256], BF16, es=es2)
        HB = kb.psum("cmpH", [128, 512], F32, es=es2)
        OB = kb.psum("cmpO", [128, 512], F32, es=es2)
        TRb = kb.psum("cmpT", [128, 1024], BF16, es=es2)
        memset(kb, "pool", gath[:], 0.0, [gath.t])
        if "pe" in steps:
            kb.dma(pe_st[:], P["nsa_pe"][l], writes=[pe_st.t])
            tcopy(kb, "dve", pe_b[:, 0:64], pe_st[:], [pe_st.t], [pe_b.t])
            tcopy(kb, "dve", pe_b[:, 64:128], pe_st[:], [pe_st.t], [pe_b.t])
            tr(kb, TRb[:, 0:32], pe_b[:], C.ident[0:32, 0:32], [pe_b.t, C.ident.t], [TRb.t])
            tcopy(kb, "dve", peT[:], TRb[:, 0:32], [], [TRb.t, peT.t])
            memset(kb, "pool", hidT[:], 0.0, [hidT.t])
        for which in ("k", "v"):
            w1n, b1n, w2n, b2n = (f"nsa_c{which}_w1", f"nsa_c{which}_b1", f"nsa_c{which}_w2", f"nsa_c{which}_b2")
            src = LS.KcT if which == "k" else LS.VcT
            if "w" in steps:
                for half in range(2):
                    for lq in range(4):
                        i_ = C.st_i = getattr(C, "st_i", 0) + 1
                        st = C.stage[i_ % len(C.stage)]
                        hp_ = slice(half * 64, (half + 1) * 64)
                        kb.dma(st[hp_, 0:1024].rearrange("p (l f) -> p l f", l=8), P[w1n][l, lq * 8:(lq + 1) * 8].rearrange("l d f -> d l f"), writes=[st.t])
                        cast_scaled(kb, C, W1[hp_, lq * 8:(lq + 1) * 8, :],
                                    st[hp_, 0:1024].rearrange("p (l f) -> p l f", l=8), None, [st.t], [W1.t])
                st = stage_load(kb, C, P[w2n][l], 128, 64)
                cast_scaled(kb, C, W2[:, 0:64], st[:, 0:64], None, [st.t], [W2.t])
                cast_scaled(kb, C, W2[:, 64:128], st[:, 0:64], None, [st.t], [W2.t])
            if "dma4" in steps:
                kb.dma(hb[:, 0:1], P[b1n][l].rearrange("(p o) -> p o", o=1), writes=[hb.t])
                kb.dma(b2c[0:64, :], P[b2n][l].rearrange("(p o) -> p o", o=1), writes=[b2c.t])
                kb.dma(b2c[64:128, :], P[b2n][l].rearrange("(p o) -> p o", o=1), writes=[b2c.t])
                kb.dma(b2bc[:], bass.AP(P[b2n].h.tensor, l * 64, [[0, 128], [1, 64]]), writes=[b2bc.t])
            if "gath" in steps:
                for lq in range(32):
                    srcv = bass.AP(src.h, src[:, lq:lq + 1].offset, [list(src[:, 0:1].ap[0]), [16, 255]])
                    tcopy(kb, ("dve", "pool", "act")[lq % 3], gath[:, lq, 0:255], srcv, [src.t], [gath.t])
                tcopy(kb, "dve", gath[:, :, 255], peT[:], [peT.t], [gath.t])
            for kv in range(2):
                lo, hi = kv * 64, (kv + 1) * 64
                first = True
                if "mm1" in steps:
                    for lq in range(32):
                        mm(kb, HB[:, 0:256], W1[lo:hi, lq, :], gath[lo:hi, lq, 0:256], first, [W1.t, gath.t], [HB.t])
                        first = False
                if "mm1" in steps:
                    tcopy(kb, "act", hx[:, 0:256], HB[:, 0:256], [], [HB.t, hx.t])
                    ts(kb, "dve", hx[:, 0:255], hx[:, 0:255], hx[:, 255:256], hb[:, 0:1], ALU.add, ALU.add, [hx.t, hb.t], [hx.t])
                if "gelu" in steps:
                    gelu_tanh(kb, hidT[:, 0:255], hx[:, 0:255], ht1[:, 0:255], ht2[:, 0:255], [hx.t], [hidT.t], ht1.t)
                if which == "k":
                    if "mm2k" in steps:
                        mm(kb, OB[:, 0:256], W2[:], hidT[:, 0:256], True, [W2.t, hidT.t], [OB.t])
                        ts(kb, "dve", KcmpT[lo:hi, kv, 0:255], OB[lo:hi, 0:255], b2c[lo:hi, 0:1], None, ALU.add, None, [b2c.t], [OB.t, KcmpT.t])
                elif "mm2v" in steps:
                    for kt in range(2):
                        n = 128
                        mm(kb, OB[0:n, kt * 64:(kt + 1) * 64], hidT[:, kt * 128:kt * 128 + n], W2[:, 0:64], kt == 0, [W2.t, hidT.t], [OB.t])
                    for kt in range(2):
                        n = 128
                        tt(kb, "dve", Vcmp[0:n, kt, kv, 0:64], OB[0:n, kt * 64:(kt + 1) * 64], b2bc[0:n, :], ALU.add, [b2bc.t], [OB.t, Vcmp.t])
        kb.barrier()
        kb.flush()
    return KcmpT, Vcmp


def nsa_attention(kb, C, l, LS):
    P = C.P
    with contextlib.ExitStack() as es:
        KcmpT, Vcmp = nsa_compress(kb, C, l, LS, es)
        import os
        if os.environ.get("NSA_COMPRESS_ONLY"):
            kb.barrier()
            kb.flush()
            return
        C.Eall = kb.sbuf("Eall", [128, S], BF16, es=es)
        memset(kb, "pool", C.Eall[:], 1.0, [C.Eall.t])
        asel(kb, C.Eall[:], C.Eall[:], [[1, S]], ALU.is_ge, 0.0, 0, -64, [C.Eall.t], [C.Eall.t])
        asel(kb, C.Eall[:], C.Eall[:], [[-1, S]], ALU.is_ge, 0.0, 63, 64, [C.Eall.t], [C.Eall.t])
        SC = [kb.psum(f"nsSC{i}", [128, 512], F32, es=es) for i in range(2)]
        OC = kb.psum("nsOC", [128, 512], F32, es=es)
        OS = kb.psum("nsOS", [128, 512], F32, es=es)
        OW = kb.psum("nsOW", [128, 512], F32, es=es)
        IM = kb.psum("nsIM", [128, 512], F32, es=es)
        TRb = kb.psum("nsTR", [128, 1024], BF16, es=es)
        PTs = [kb.sbuf(f"PT{i}", [128, 384], BF16, es=es) for i in range(4)]
        oc = kb.sbuf("oc", [128, 6, 66], F32, es=es)
        osb = kb.sbuf("osb", [128, 6, 66], F32, es=es)
        owb = kb.sbuf("owb", [128, 6, 66], F32, es=es)
        rc = kb.sbuf("rc", [128, 3, 6], F32, es=es)
        imp = kb.sbuf("imp", [128, 2, 64], F32, es=es)
        imp2 = kb.sbuf("imp2", [128, 2, 64], F32, es=es)
        m8 = kb.sbuf("m8", [128, 2, 16], F32, es=es)
        negm = kb.sbuf("negm", [128, 2, 64], BF16, es=es)
        negmT = kb.sbuf("negmT", [128, 2, 3, 128], BF16, es=es)
        memset(kb, "pool", negmT[:], 0.0, [negmT.t])
        oa = kb.sbuf("oa", [128, 6, 64], F32, es=es)
        otmp = kb.sbuf("otmp", [128, 6, 64], F32, es=es)
        oab = kb.sbuf("oab", [128, 384], BF16, es=es)
        oaTs = [kb.sbuf(f"oaT{i}", [128, 3, 128], BF16, es=es) for i in range(2)]
        pti = [0]
        sci = [0]

        def unit(score_fn, ncols, scale, mask, pv_fn):
            bank = SC[sci[0] % 2]
            sci[0] += 1
            PT = PTs[pti[0] % 4]
            pti[0] += 1
            score_fn(bank)
            act(kb, PT[:, 0:ncols], bank[:, 0:ncols], AF.Exp, [], [bank.t, PT.t], scale=scale)
            if mask is not None:
                base, cm, step = mask
                asel(kb, PT[:, 0:ncols].rearrange("p (g q) -> p g q", q=128), PT[:, 0:ncols].rearrange("p (g q) -> p g q", q=128),
                     [[0, ncols // 128], [step, 128]], ALU.is_ge, 0.0, base, cm, [PT.t], [PT.t])
            pv_fn(PT)

        for i in range(NT):
            qc = slice(i * 128, (i + 1) * 128)
            nkt = 1 if (8 * i + 6) < 128 else 2
            firstC = [True]
            firstI = [True]
            for kv in range(2):
                lo, hi = kv * 64, (kv + 1) * 64
                for kt in range(nkt):
                    def score_fn(bank, kt=kt, lo=lo, hi=hi, kv=kv):
                        mm(kb, bank[:, 0:384], KcmpT[:, kv, kt * 128:(kt + 1) * 128], LS.QaT[:, :, qc], True, [KcmpT.t, LS.QaT.t], [bank.t])

                    def pv_fn(PT, kt=kt, kv=kv):
                        for g in range(3):
                            h = kv * 3 + g
                            mm(kb, OC[:, h * 80:h * 80 + 66], PT[:, g * 128:(g + 1) * 128], Vcmp[:, kt, kv, :], firstC[0], [PT.t, Vcmp.t], [OC.t])
                            firstC[0] = False
                            mm(kb, IM[:, h * 64:(h + 1) * 64], PT[:, g * 128:(g + 1) * 128], C.Ov[:, kt, :], firstI[0], [PT.t, C.Ov.t], [IM.t])
                            firstI[0] = False
                    unit(score_fn, 384, 0.125, (128 * i - 2048 * kt - 31, -16, 1), pv_fn)
            tcopy(kb, "act", oc[:], OC[:, 0:480].rearrange("p (h d) -> p h d", h=6)[:, :, 0:66], [], [OC.t, oc.t])
            ts(kb, "dve", rc[:, 0, :], oc[:, :, 64], 1e-30, None, ALU.max, None, [oc.t], [rc.t])
            kb.op("dve", lambda e: e.reciprocal(out=rc[:, 0, :], in_=rc[:, 0, :]), [rc.t], [rc.t])
            for kv in range(2):
                ts(kb, "dve", imp[:, kv, :], IM[:, (kv * 3) * 64:(kv * 3 + 1) * 64], rc[:, 0, kv * 3:kv * 3 + 1], None, ALU.mult, None, [rc.t], [IM.t, imp.t])
                for g in (1, 2):
                    h = kv * 3 + g
                    stt(kb, imp[:, kv, :], IM[:, h * 64:(h + 1) * 64], rc[:, 0, h:h + 1], imp[:, kv, :], ALU.mult, ALU.add, [rc.t], [IM.t, imp.t])
            for hf in range(2):
                cur = 2 * i + hf
                pr = slice(hf * 64, (hf + 1) * 64)
                if cur + 1 < 64:
                    memset(kb, "pool", imp[pr, :, cur + 1:64], -1.0, [imp.t])
                memset(kb, "pool", imp[pr, :, 0:1], 100.0, [imp.t])
                memset(kb, "pool", imp[pr, :, max(cur - 1, 0):cur + 1], 100.0, [imp.t])
            for kv in range(2):
                kb.op("dve", lambda e, kv=kv: e.max(out=m8[:, kv, 0:8], in_=imp[:, kv, :]), [imp.t], [m8.t])
                kb.op("dve", lambda e, kv=kv: e.match_replace(out=imp2[:, kv, :], in_to_replace=m8[:, kv, 0:8], in_values=imp[:, kv, :], imm_value=-1e30), [imp.t, m8.t], [imp2.t])
                kb.op("dve", lambda e, kv=kv: e.max(out=m8[:, kv, 8:16], in_=imp2[:, kv, :]), [imp2.t], [m8.t])
                ts(kb, "dve", negm[:, kv, :], imp[:, kv, :], m8[:, kv, 15:16], NEG, ALU.is_lt, ALU.mult, [imp.t, m8.t], [negm.t])
                tr(kb, TRb[0:64, kv * 128:(kv + 1) * 128], negm[:, kv, :], C.ident[:], [negm.t, C.ident.t], [TRb.t])
            for g in range(3):
                tcopy(kb, "dve" if g != 1 else "act", negmT[0:64, :, g, :], TRb[0:64, 0:256].rearrange("p (k q) -> p k q", k=2), [], [TRb.t, negmT.t])
            firstS = [True]
            for kv in range(2):
                lo, hi = kv * 64, (kv + 1) * 64
                for j in range(i + 1):
                    kc = slice(j * 128, (j + 1) * 128)

                    def score_fn(bank, kc=kc, lo=lo, hi=hi, kv=kv):
                        mm(kb, bank[:, 0:384], LS.KsT[:, kv, kc], LS.QaT[:, :, qc], True, [LS.KsT.t, LS.QaT.t], [bank.t])
                        mm(kb, bank[:, 0:384], C.Eall[:, kc], negmT[:, kv, :, :], False, [C.Eall.t, negmT.t], [bank.t])

                    def pv_fn(PT, j=j, kv=kv):
                        for g in range(3):
                            h = kv * 3 + g
                            mm(kb, OS[:, h * 80:h * 80 + 66], PT[:, g * 128:(g + 1) * 128], LS.Vs[:, j, kv, :], firstS[0], [PT.t, LS.Vs.t], [OS.t])
                            firstS[0] = False
                    unit(score_fn, 384, 0.125, (0, -1, 1) if j == i else None, pv_fn)
            firstW = [True]
            for kv in range(2):
                lo, hi = kv * 64, (kv + 1) * 64
                for j in range(max(0, i - 4), i + 1):
                    kc = slice(j * 128, (j + 1) * 128)

                    def score_fn(bank, kc=kc, lo=lo, hi=hi, kv=kv):
                        mm(kb, bank[:, 0:384], LS.KwT[:, kv, kc], LS.QaT[:, :, qc], True, [LS.KwT.t, LS.QaT.t], [bank.t])

                    def pv_fn(PT, j=j, kv=kv):
                        for g in range(3):
                            h = kv * 3 + g
                            mm(kb, OW[:, h * 80:h * 80 + 66], PT[:, g * 128:(g + 1) * 128], LS.Vw[:, j, kv, :], firstW[0], [PT.t, LS.Vw.t], [OW.t])
                            firstW[0] = False
                    mask = None
                    if j == i:
                        mask = (0, -1, 1)
                    elif j == i - 4:
                        mask = (-1, 1, -1)
                    unit(score_fn, 384, 0.125, mask, pv_fn)
            tcopy(kb, "act", osb[:], OS[:, 0:480].rearrange("p (h d) -> p h d", h=6)[:, :, 0:66], [], [OS.t, osb.t])
            tcopy(kb, "act", owb[:], OW[:, 0:480].rearrange("p (h d) -> p h d", h=6)[:, :, 0:66], [], [OW.t, owb.t])
            for bi, ob in ((1, osb), (2, owb)):
                ts(kb, "dve", rc[:, bi, :], ob[:, :, 64], 1e-30, None, ALU.max, None, [ob.t], [rc.t])
                kb.op("dve", lambda e, bi=bi: e.reciprocal(out=rc[:, bi, :], in_=rc[:, bi, :]), [rc.t], [rc.t])
            tt(kb, "dve", rc[:], rc[:], LS.Gt[:, i, :].rearrange("p (h b) -> p b h", b=3), ALU.mult, [rc.t, LS.Gt.t], [rc.t])
            tt(kb, "dve", oa[:], oc[:, :, 0:64], bcast(rc[:, 0, :], [(2, 64)]), ALU.mult, [oc.t, rc.t], [oa.t])
            for bi, ob in ((1, osb), (2, owb)):
                tt(kb, "dve", otmp[:], ob[:, :, 0:64], bcast(rc[:, bi, :], [(2, 64)]), ALU.mult, [ob.t, rc.t], [otmp.t])
                tt(kb, "dve", oa[:], oa[:], otmp[:], ALU.add, [oa.t, otmp.t], [oa.t])
            ra = rstd_of(kb, C, oa[:].rearrange("p h d -> p (h d)"), 384, [oa.t], None)
            ts(kb, "dve", oab[:], oa[:].rearrange("p h d -> p (h d)"), ra[:, 0:1], None, ALU.mult, None, [oa.t, ra.t], [oab.t])
            oaT = oaTs[i % 2]
            for c in range(3):
                tr(kb, TRb[:, 256 + c * 128:256 + (c + 1) * 128], oab[:, c * 128:(c + 1) * 128], C.ident[:], [oab.t, C.ident.t], [TRb.t])
            tcopy(kb, "act", oaT[:], TRb[:, 256:640].rearrange("p (c t) -> p c t", c=3), [], [TRb.t, oaT.t])
            kb.dma(C.MIXT[i, :, 0:3, :], oaT[:], reads=[oaT.t], writes=[C.MIXT.t])
        kb.barrier()
        kb.flush()


def mla_phase(kb, C, l):
    P = C.P
    sc = 96.0 ** -0.5
    with contextlib.ExitStack() as es:
        KnT = kb.sbuf("KnT", [128, 6, S], BF16, es=es)
        memset(kb, "pool", KnT[:], 0.0, [KnT.t])
        QnT = kb.sbuf("QnT", [128, 3, S], BF16, es=es)
        KrT = kb.sbuf("KrT", [128, S], BF16, es=es)
        memset(kb, "pool", KrT[:], 0.0, [KrT.t])
        Vm = kb.sbuf("Vm", [128, NT, 6, 66], BF16, es=es)
        memset(kb, "pool", Vm[:], 1.0, [Vm.t])
        with contextlib.ExitStack() as es2:
            make_stage(kb, C, es2, 1024)
            posf = kb.sbuf("posf", [96, S], F32, es=es2)
            posi = kb.sbuf("posi", [96, S], I32, es=es2)
            kb.dma(posi[:], bass.AP(C.pos_d.h.tensor, 0, [[0, 96], [1, S]]), writes=[posi.t])
            tcopy(kb, "dve", posf[:], posi[:], [posi.t], [posf.t])
            ts(kb, "dve", posf[:], posf[:], C.invf[:, 0:1], 1.0 / TWO_PI, ALU.mult, ALU.mult, [posf.t, C.invf.t], [posf.t])
            C.posf = posf
            Wq = kb.sbuf("Wq", [128, 3, 576], BF16, es=es2)
            Wk = kb.sbuf("Wk", [128, 384], BF16, es=es2)
            Wv = kb.sbuf("Wv", [128, 384], BF16, es=es2)
            qg = kb.sbuf("qg", [128, 4], F32, es=es2)
            PB = [kb.psum(f"mlP{i}", [128, 512], F32, es=es2) for i in range(4)]
            load_cols(kb, C, qg[:, 0:3], qg.t, P["mla_q_norm"][l], 3, PB[0])
            kb.dma(qg[:, 3:4], P["mla_kv_norm"][l].rearrange("(p o) -> p o", o=1), writes=[qg.t])
            for kc in range(3):
                st = stage_load(kb, C, P["mla_w_uq"][l, kc * 128:(kc + 1) * 128, :], 128, 576)
                sv = st[:, 0:576].rearrange("p (h e) -> p h e", h=6)
                g = qg[:, kc:kc + 1]
                cast_scaled(kb, C, Wq[:, kc, 0:384].rearrange("p (h e) -> p h e", h=6), sv[:, :, 0:64], g, [st.t, qg.t], [Wq.t])
                cast_scaled(kb, C, Wq[:, kc, 384:480].rearrange("p (h e) -> p h e", h=6), sv[:, :, 64:80], g, [st.t, qg.t], [Wq.t])
                cast_scaled(kb, C, Wq[:, kc, 480:576].rearrange("p (h e) -> p h e", h=6), sv[:, :, 80:96], g, [st.t, qg.t], [Wq.t])
            st = stage_load(kb, C, P["mla_w_uk"][l], 128, 384)
            cast_scaled(kb, C, Wk[:], st[:, 0:384], qg[:, 3:4], [st.t, qg.t], [Wk.t])
            st = stage_load(kb, C, P["mla_w_uv"][l], 128, 384)
            cast_scaled(kb, C, Wv[:], st[:, 0:384], qg[:, 3:4], [st.t, qg.t], [Wv.t])
            lat = [kb.sbuf(f"mlat{i}", [128, 4, 512], BF16, es=es2) for i in range(1)]
            kr_raw = [kb.sbuf(f"krr{i}", [16, 2, 512], F32, es=es2) for i in range(1)]
            tb = [kb.sbuf(f"mtb{i}", [96, 512], F32, es=es2) for i in range(2)]
            tbi = kb.sbuf("mtbi", [96, 512], I32, es=es2)
            sn = [kb.sbuf(f"msn{i}", [96, 512], F32, es=es2) for i in range(2)]
            cs = [kb.sbuf(f"mcs{i}", [96, 512], F32, es=es2) for i in range(2)]
            x1 = kb.sbuf("mx1", [96, 512], F32, es=es2)
            x2 = kb.sbuf("mx2", [96, 512], F32, es=es2)
            r1 = kb.sbuf("mr1", [96, 512], F32, es=es2)
            r2 = kb.sbuf("mr2", [96, 512], F32, es=es2)
            qo = [kb.sbuf(f"mqo{i}", [96, 2, 512], BF16, es=es2) for i in range(2)]
            ko = [kb.sbuf(f"mko{i}", [16, 2, 512], BF16, es=es2) for i in range(2)]
            pbi = 0
            for tg in range(8):
                cols = slice(tg * 512, (tg + 1) * 512)
                la = lat[0]
                kb.dma(la[:], C.LATT[:, cols].rearrange("(c p) t -> p c t", p=128), reads=[C.LATT.t], writes=[la.t])
                kr = kr_raw[0]
                kb.dma(kr[:], C.KR12[:, cols].rearrange("(two r) t -> r two t", two=2), reads=[C.KR12.t], writes=[kr.t])
                tbt = tb[tg % 2]
                snt = sn[tg % 2]
                cst = cs[tg % 2]
                tcopy(kb, "pool", tbt[:], C.posf[:, cols], [C.posf.t], [tbt.t])
                sincos_from_turns(kb, tbt[:], snt[:], cst[:], tbi[:], tbt.t, [])
                trk = tbt.t
                for c in range(3):
                    bank = PB[pbi % 4]; pbi += 1
                    mm(kb, bank[:, :], Wk[:, c * 128:(c + 1) * 128], la[:, 3, :], True, [Wk.t, la.t], [bank.t])
                    tcopy(kb, "act", KnT[0:64, 2 * c, cols], bank[0:64, :], [], [bank.t, KnT.t])
                    tcopy(kb, "dve", KnT[64:128, 2 * c + 1, cols], bank[64:128, :], [], [bank.t, KnT.t])
                for c in range(3):
                    bank = PB[pbi % 4]; pbi += 1
                    for kc in range(3):
                        mm(kb, bank[:, :], Wq[:, kc, c * 128:(c + 1) * 128], la[:, kc, :], kc == 0, [Wq.t, la.t], [bank.t])
                    tcopy(kb, "dve" if c % 2 else "act", QnT[:, c, cols], bank[:, :], [], [bank.t, QnT.t])
                for tl in range(4):
                    bank = PB[pbi % 4]; pbi += 1
                    mm(kb, bank[:, 0:384], la[:, 3, tl * 128:(tl + 1) * 128], Wv[:], True, [Wv.t, la.t], [bank.t])
                    tcopy(kb, "act", Vm[:, tg * 4 + tl, :, 0:64], bank[:, 0:384].rearrange("p (h d) -> p h d", h=6), [], [bank.t, Vm.t])
                b1 = PB[pbi % 4]; pbi += 1
                b2 = PB[pbi % 4]; pbi += 1
                for kc in range(3):
                    mm(kb, b1[0:96, :], Wq[:, kc, 384:480], la[:, kc, :], kc == 0, [Wq.t, la.t], [b1.t])
                for kc in range(3):
                    mm(kb, b2[0:96, :], Wq[:, kc, 480:576], la[:, kc, :], kc == 0, [Wq.t, la.t], [b2.t])
                tcopy(kb, "act", x1[:], b1[0:96, :], [], [b1.t, x1.t])
                tcopy(kb, "act", x2[:], b2[0:96, :], [], [b2.t, x2.t])
                q_o = qo[tg % 2]
                tt(kb, "dve", r1[:], x1[:], cst[:], ALU.mult, [x1.t, trk], [r1.t])
                tt(kb, "pool", r2[:], x2[:], snt[:], ALU.mult, [x2.t, trk], [r2.t])
                tt(kb, "dve", q_o[:, 0, :], r1[:], r2[:], ALU.subtract, [r1.t, r2.t], [q_o.t])
                tt(kb, "dve", r1[:], x2[:], cst[:], ALU.mult, [x2.t, trk], [r1.t])
                tt(kb, "pool", r2[:], x1[:], snt[:], ALU.mult, [x1.t, trk], [r2.t])
                tt(kb, "dve", q_o[:, 1, :], r1[:], r2[:], ALU.add, [r1.t, r2.t], [q_o.t])
                for two in range(2):
                    kb.dma(C.QRD[two, :, :, cols].rearrange("h r t -> (h r) t"), q_o[:, two, :], reads=[q_o.t], writes=[C.QRD.t])
                k_o = ko[tg % 2]
                tt(kb, "dve", r1[0:16, :], kr[:, 0, :], cst[0:16, :], ALU.mult, [kr.t, trk], [r1.t])
                tt(kb, "pool", r2[0:16, :], kr[:, 1, :], snt[0:16, :], ALU.mult, [kr.t, trk], [r2.t])
                tt(kb, "dve", k_o[:, 0, :], r1[0:16, :], r2[0:16, :], ALU.subtract, [r1.t, r2.t], [k_o.t])
                tt(kb, "dve", r1[0:16, :], kr[:, 1, :], cst[0:16, :], ALU.mult, [kr.t, trk], [r1.t])
                tt(kb, "pool", r2[0:16, :], kr[:, 0, :], snt[0:16, :], ALU.mult, [kr.t, trk], [r2.t])
                tt(kb, "dve", k_o[:, 1, :], r1[0:16, :], r2[0:16, :], ALU.add, [r1.t, r2.t], [k_o.t])
                kb.dma(C.KRD[:, cols].rearrange("(two r) t -> r two t", two=2), k_o[:], reads=[k_o.t], writes=[C.KRD.t])
            kb.barrier()
            kb.flush()
        import os
        if os.environ.get("MLA_PART") == "A":
            return
        mlab = os.environ.get("MLA_B", "sc,asel,pv,fin").split(",")
        kb.dma(KrT[0:32, :], C.KRD[:], reads=[C.KRD.t], writes=[KrT.t])
        SC = [kb.psum(f"mlSC{i}", [128, 512], F32, es=es) for i in range(3)]
        OB = [kb.psum(f"mlO{i}", [128, 512], F32, es=es) for i in range(2)]
        TRb = kb.psum("mlTR", [128, 1024], BF16, es=es)
        PTs = [kb.sbuf(f"mPT{i}", [128, 384], BF16, es=es) for i in range(4)]
        QrTs = [kb.sbuf(f"QrT{i}", [128, 6, 128], BF16, es=es) for i in range(2)]
        for t_ in QrTs:
            memset(kb, "pool", t_[:], 0.0, [t_.t])
        ob = kb.sbuf("mob", [128, 6, 66], F32, es=es)
        rcp = kb.sbuf("mrc", [128, 6], F32, es=es)
        on = kb.sbuf("mon", [128, 6, 64], F32, es=es)
        onb = kb.sbuf("monb", [128, 384], BF16, es=es)
        obTs = [kb.sbuf(f"mobT{i}", [128, 3, 128], BF16, es=es) for i in range(2)]
        u = 0
        for i in range(NT):
            qc = slice(i * 128, (i + 1) * 128)
            QrT = QrTs[i % 2]
            for two in range(2):
                kb.dma(QrT[two * 16:(two + 1) * 16, :, :], C.QRD[two, :, :, qc].rearrange("h r t -> r h t"), reads=[C.QRD.t], writes=[QrT.t])
            O = OB[i % 2]
            first = True
            for grp in range(2):
                for j in range(i + 1):
                    kc = slice(j * 128, (j + 1) * 128)
                    bank = SC[u % 3]
                    PT = PTs[u % 4]
                    u += 1
                    if "sc" not in mlab:
                        continue
                    for g in range(3):
                        h = grp * 3 + g
                        c, hh = h // 2, h % 2
                        pr = slice(hh * 64, (hh + 1) * 64)
                        mm(kb, bank[:, g * 128:(g + 1) * 128], KnT[:, h, kc], QnT[:, c, qc], g == 0, [KnT.t, QnT.t], [bank.t])
                    mm(kb, bank[:, 0:384], KrT[:, kc], QrT[:, grp * 3:(grp + 1) * 3, :], False, [KrT.t, QrT.t], [bank.t])
                    act(kb, PT[:, 0:384], bank[:, 0:384], AF.Exp, [], [bank.t, PT.t], scale=sc)
                    if j == i and "asel" in mlab:
                        asel(kb, PT[:].rearrange("p (g q) -> p g q", q=128), PT[:].rearrange("p (g q) -> p g q", q=128),
                             [[0, 3], [1, 128]], ALU.is_ge, 0.0, 0, -1, [PT.t], [PT.t])
                    for g in range(3):
                        if "pv" not in mlab:
                            break
                        h = grp * 3 + g
                        mm(kb, O[:, h * 80:h * 80 + 66], PT[:, g * 128:(g + 1) * 128], Vm[:, j, h, :], first, [PT.t, Vm.t], [O.t])
                        first = False
            if "fin" not in mlab:
                continue
            tcopy(kb, "act", ob[:], O[:, 0:480].rearrange("p (h d) -> p h d", h=6)[:, :, 0:66], [], [O.t, ob.t])
            kb.op("dve", lambda e: e.reciprocal(out=rcp[:], in_=ob[:, :, 64]), [ob.t], [rcp.t])
            tt(kb, "dve", on[:], ob[:, :, 0:64], bcast(rcp[:], [(2, 64)]), ALU.mult, [ob.t, rcp.t], [on.t])
            rb = rstd_of(kb, C, on[:].rearrange("p h d -> p (h d)"), 384, [on.t], None)
            ts(kb, "dve", onb[:], on[:].rearrange("p h d -> p (h d)"), rb[:, 0:1], None, ALU.mult, None, [on.t, rb.t], [onb.t])
            obT = obTs[i % 2]
            for c in range(3):
                tr(kb, TRb[:, c * 128:(c + 1) * 128], onb[:, c * 128:(c + 1) * 128], C.ident[:], [onb.t, C.ident.t], [TRb.t])
            tcopy(kb, "act", obT[:], TRb[:, 0:384].rearrange("p (c t) -> p c t", c=3), [], [TRb.t, obT.t])
            kb.dma(C.MIXT[i, :, 3:6, :], obT[:], reads=[obT.t], writes=[C.MIXT.t])
        kb.barrier()
        kb.flush()


def ssm_phase(kb, C, l):
    P = C.P
    TB = 1024
    with contextlib.ExitStack() as es:
        make_stage(kb, C, es, 512)
        TRb = kb.psum("sTR", [128, 1024], BF16, es=es)
        BR = [kb.psum(f"sBR{i}", [128, 512], F32, es=es) for i in range(2)]
        BI = [kb.psum(f"sBI{i}", [128, 512], F32, es=es) for i in range(2)]
        Y = [kb.psum(f"sY{i}", [128, 512], F32, es=es) for i in range(2)]
        UTs = kb.sbuf("UTs", [128, 2, S], BF16, es=es)
        zT = kb.sbuf("zT", [128, 2, S], BF16, es=es)
        kb.dma(UTs[:], C.UT[:].rearrange("(c p) t -> p c t", p=128), reads=[C.UT.t], writes=[UTs.t])
        def pl(name):
            return P[name][l].rearrange("(q two) p -> two p q", two=2)
        prm = kb.sbuf("sprm", [128, 24, 8], F32, es=es)
        pt = prm.t
        (DT, ARE, AIM, LRE, TH, R, CO, SI, NR, DEN, T1, T2, CRE, CIM, FT, TI) = range(16)
        load_cols(kb, C, prm[:, ARE, :], pt, P["ssm_a_re"][l].rearrange("g p -> (g p)"), 8, Y[0])
        load_cols(kb, C, prm[:, AIM, :], pt, P["ssm_a_im"][l].rearrange("g p -> (g p)"), 8, Y[1])
        ldt = kb.sbuf("sldt", [128, 16], F32, es=es)
        kb.dma(ldt[:], bass.AP(P["ssm_log_dt"].h.tensor, l * 16, [[0, 128], [1, 16]]), writes=[ldt.t])
        for two in range(2):
            pr = slice(two * 64, (two + 1) * 64)
            tcopy(kb, "dve", prm[pr, DT, :], ldt[pr, :].rearrange("p (q two) -> p two q", two=2)[:, two, :], [ldt.t], [pt])
        act(kb, prm[:, DT, :], prm[:, DT, :], AF.Exp, [pt], [pt])
        tt(kb, "dve", prm[:, LRE, :], prm[:, ARE, :], prm[:, DT, :], ALU.mult, [pt], [pt])
        tt(kb, "dve", prm[:, TH, :], prm[:, AIM, :], prm[:, DT, :], ALU.mult, [pt], [pt])
        act(kb, prm[:, R, :], prm[:, LRE, :], AF.Exp, [pt], [pt])
        ts(kb, "dve", prm[:, FT, :], prm[:, TH, :], 1.0 / TWO_PI, None, ALU.mult, None, [pt], [pt])
        tcopy(kb, "dve", prm[:, T1, :], prm[:, FT, :], [pt], [pt])
        ti = kb.sbuf("sti", [128, 8], I32, es=es)
        sincos_from_turns(kb, prm[:, T1, :], prm[:, SI, :], prm[:, CO, :], ti[:], pt, [])
        tt(kb, "dve", prm[:, NR, :], prm[:, R, :], prm[:, CO, :], ALU.mult, [pt], [pt])
        ts(kb, "dve", prm[:, NR, :], prm[:, NR, :], -1.0, None, ALU.add, None, [pt], [pt])
        tt(kb, "dve", prm[:, T1, :], prm[:, R, :], prm[:, SI, :], ALU.mult, [pt], [pt])
        tt(kb, "dve", prm[:, DEN, :], prm[:, ARE, :], prm[:, ARE, :], ALU.mult, [pt], [pt])
        tt(kb, "dve", prm[:, T2, :], prm[:, AIM, :], prm[:, AIM, :], ALU.mult, [pt], [pt])
        tt(kb, "dve", prm[:, DEN, :], prm[:, DEN, :], prm[:, T2, :], ALU.add, [pt], [pt])
        kb.op("dve", lambda e: e.reciprocal(out=prm[:, DEN, :], in_=prm[:, DEN, :]), [pt], [pt])
        tt(kb, "dve", prm[:, CRE, :], prm[:, NR, :], prm[:, ARE, :], ALU.mult, [pt], [pt])
        tt(kb, "dve", prm[:, T2, :], prm[:, T1, :], prm[:, AIM, :], ALU.mult, [pt], [pt])
        tt(kb, "dve", prm[:, CRE, :], prm[:, CRE, :], prm[:, T2, :], ALU.add, [pt], [pt])
        tt(kb, "dve", prm[:, CRE, :], prm[:, CRE, :], prm[:, DEN, :], ALU.mult, [pt], [pt])
        tt(kb, "dve", prm[:, CIM, :], prm[:, T1, :], prm[:, ARE, :], ALU.mult, [pt], [pt])
        tt(kb, "dve", prm[:, T2, :], prm[:, NR, :], prm[:, AIM, :], ALU.mult, [pt], [pt])
        tt(kb, "dve", prm[:, CIM, :], prm[:, CIM, :], prm[:, T2, :], ALU.subtract, [pt], [pt])
        tt(kb, "dve", prm[:, CIM, :], prm[:, CIM, :], prm[:, DEN, :], ALU.mult, [pt], [pt])
        Braw = kb.sbuf("sBraw", [128, 2, 8, 16], F32, es=es)
        for ri, nm in enumerate(("ssm_b_re", "ssm_b_im")):
            for two in range(2):
                kb.dma(Braw[two * 64:(two + 1) * 64, ri, :, :], P[nm][l].rearrange("(q two) p c -> two p q c", two=2)[two], writes=[Braw.t])
        bbr = kb.sbuf("sbbr", [128, 8, 16], F32, es=es)
        bbi = kb.sbuf("sbbi", [128, 8, 16], F32, es=es)
        btmp = kb.sbuf("sbtmp", [128, 8, 16], F32, es=es)
        cre_b = bcast(prm[:, CRE, :], [(2, 16)])
        cim_b = bcast(prm[:, CIM, :], [(2, 16)])
        tt(kb, "dve", bbr[:], Braw[:, 0], cre_b, ALU.mult, [Braw.t, pt], [bbr.t])
        tt(kb, "dve", btmp[:], Braw[:, 1], cim_b, ALU.mult, [Braw.t, pt], [btmp.t])
        tt(kb, "dve", bbr[:], bbr[:], btmp[:], ALU.subtract, [bbr.t, btmp.t], [bbr.t])
        tt(kb, "dve", bbi[:], Braw[:, 1], cre_b, ALU.mult, [Braw.t, pt], [bbi.t])
        tt(kb, "dve", btmp[:], Braw[:, 0], cim_b, ALU.mult, [Braw.t, pt], [btmp.t])
        tt(kb, "dve", bbi[:], bbi[:], btmp[:], ALU.add, [bbi.t, btmp.t], [bbi.t])
        Zb = kb.sbuf("sZb", [128, 128], BF16, es=es)
        LB = kb.sbuf("sLB", [128, 2, 8, 128], BF16, es=es)
        LC = kb.sbuf("sLC", [128, 4, 8, 128], BF16, es=es)
        for ri, bb in enumerate((bbr, bbi)):
            for q in range(8):
                memset(kb, "pool", Zb[:], 0.0, [Zb.t])
                c0 = 32 * (q % 4)
                tcopy(kb, "dve", Zb[0:64, c0:c0 + 16], bb[0:64, q, :], [bb.t], [Zb.t])
                tcopy(kb, "dve", Zb[64:128, c0 + 16:c0 + 32], bb[64:128, q, :], [bb.t], [Zb.t])
                tr(kb, TRb[:, 0:128], Zb[:], C.ident[:], [Zb.t, C.ident.t], [TRb.t])
                tcopy(kb, "act", LB[:, ri, q, :], TRb[:, 0:128], [], [TRb.t, LB.t])
        memset(kb, "pool", LC[:], 0.0, [LC.t])
        Xc = kb.sbuf("sXc", [128, 128], F32, es=es)
        Xcb = kb.sbuf("sXcb", [128, 128], BF16, es=es)
        XT = kb.sbuf("sXT", [128, 128], BF16, es=es)
        XTn = kb.sbuf("sXTn", [128, 128], BF16, es=es)
        for ri, nm in enumerate(("ssm_c_re", "ssm_c_im")):
            for q in range(8):
                kb.dma(Xc[q * 16:(q + 1) * 16, :].rearrange("p (two s) -> p two s", two=2),
                       P[nm][l, 2 * q:2 * q + 2].rearrange("two c p -> c two p"), writes=[Xc.t])
            tcopy(kb, "dve", Xcb[:], Xc[:], [Xc.t], [Xcb.t])
            tr(kb, TRb[:, 128:256], Xcb[:], C.ident[:], [Xcb.t, C.ident.t], [TRb.t])
            tcopy(kb, "act", XT[:], TRb[:, 128:256], [], [TRb.t, XT.t])
            ts(kb, "dve", XTn[:], XT[:], -1.0, None, ALU.mult, None, [XT.t], [XTn.t])
            for q in range(8):
                c0 = 32 * (q % 4)
                for two in range(2):
                    pr = slice(two * 64, (two + 1) * 64)
                    dst = slice(c0 + 16 * two, c0 + 16 * two + 16)
                    if ri == 0:
                        tcopy(kb, "dve", LC[pr, 0, q, dst], XT[pr, q * 16:(q + 1) * 16], [XT.t], [LC.t])
                        tcopy(kb, "pool", LC[pr, 1, q, dst], XTn[pr, q * 16:(q + 1) * 16], [XTn.t], [LC.t])
                    else:
                        tcopy(kb, "dve", LC[pr, 2, q, dst], XTn[pr, q * 16:(q + 1) * 16], [XTn.t], [LC.t])
        dcol = kb.sbuf("sdcol", [128, 4], F32, es=es)
        load_cols(kb, C, dcol[:, 0:2], dcol.t, P["ssm_d"][l].rearrange("g c -> (g c)"), 2, Y[0])
        load_cols(kb, C, dcol[:, 2:4], dcol.t, P["ssm_b_glu"][l], 2, Y[1])
        Wg = kb.sbuf("sWg", [128, 2, 256], BF16, es=es)
        for kc in range(2):
            st = stage_load(kb, C, P["ssm_w_glu"][l, kc * 128:(kc + 1) * 128, :], 128, 256)
            cast_scaled(kb, C, Wg[:, kc, :], st[:, 0:256], None, [st.t], [Wg.t])
        tidx = kb.sbuf("stidx", [128, TB], F32, es=es)
        kb.op("pool", lambda e: e.iota(tidx[:], [[1, TB]], base=0, channel_multiplier=0, allow_small_or_imprecise_dtypes=True), [], [tidx.t])
        rdecs = [kb.sbuf(f"srdec{i}", [128, TB], F32, es=es) for i in range(2)]
        basef = kb.sbuf("sbasef", [128, S // TB, 8], F32, es=es)
        basei = kb.sbuf("sbasei", [128, S // TB, 8], I32, es=es)
        for tbk in range(S // TB):
            ts(kb, "dve", basef[:, tbk, :], prm[:, FT, :], float(tbk * TB), None, ALU.mult, None, [pt], [basef.t])
        tcopy(kb, "dve", basei[:], basef[:], [basef.t], [basei.t])
        tt(kb, "dve", basef[:], basef[:], basei[:], ALU.subtract, [basef.t, basei.t], [basef.t])
        xs = [kb.sbuf(f"sx{i}", [128, TB], F32, es=es) for i in range(2)]
        xi = kb.sbuf("sxi", [128, TB], I32, es=es)
        sns = [kb.sbuf(f"ssn{i}", [128, TB], F32, es=es) for i in range(2)]
        css = [kb.sbuf(f"scs{i}", [128, TB], F32, es=es) for i in range(2)]
        bre = kb.sbuf("sbre", [128, TB], F32, es=es)
        bim = kb.sbuf("sbim", [128, TB], F32, es=es)
        ta = kb.sbuf("sta", [128, TB], F32, es=es)
        tb_ = kb.sbuf("stb", [128, TB], F32, es=es)
        tc_ = kb.sbuf("stc", [128, TB], F32, es=es)
        td = kb.sbuf("std", [128, TB], F32, es=es)
        gre = kb.sbuf("sgre", [128, TB], F32, es=es)
        gim = kb.sbuf("sgim", [128, TB], F32, es=es)
        G = [kb.sbuf(f"sG{i}", [128, 4, TB], BF16, es=es) for i in range(2)]
        state = kb.sbuf("sstate", [128, 8, 2], F32, es=es)
        memset(kb, "pool", state[:], 0.0, [state.t])
        yb = kb.sbuf("syb", [128, TB], F32, es=es)
        y1 = kb.sbuf("sy1", [128, TB], F32, es=es)
        y2 = kb.sbuf("sy2", [128, TB], F32, es=es)
        it = 0
        for ch in range(2):
            for tbk in range(S // TB):
                t0 = tbk * TB
                for qq in range(4):
                    q = ch * 4 + qq
                    x = xs[it % 2]; snt = sns[it % 2]; cst = css[it % 2]; rdec = rdecs[it % 2]
                    it += 1
                    ts(kb, "pool", rdec[:], tidx[:], 0.0, prm[:, R, q:q + 1], ALU.mult, ALU.add, [tidx.t, pt], [rdec.t])
                    ts(kb, "dve", x[:], tidx[:], prm[:, FT, q:q + 1], basef[:, tbk, q:q + 1], ALU.mult, ALU.add, [tidx.t, pt, basef.t], [x.t])
                    sincos_from_turns(kb, x[:], snt[:], cst[:], xi[:], x.t, [])
                    trk = x.t
                    for hb_ in range(2):
                        tc0 = t0 + hb_ * 512
                        mm(kb, BR[hb_][:, :], LB[:, 0, q, :], UTs[:, ch, tc0:tc0 + 512], True, [LB.t, UTs.t], [BR[hb_].t])
                        mm(kb, BI[hb_][:, :], LB[:, 1, q, :], UTs[:, ch, tc0:tc0 + 512], True, [LB.t, UTs.t], [BI[hb_].t])
                        tcopy(kb, "act", bre[:, hb_ * 512:(hb_ + 1) * 512], BR[hb_][:, :], [], [BR[hb_].t, bre.t])
                        tcopy(kb, "act", bim[:, hb_ * 512:(hb_ + 1) * 512], BI[hb_][:, :], [], [BI[hb_].t, bim.t])
                    tt(kb, "dve", ta[:], bre[:], cst[:], ALU.mult, [bre.t, trk], [ta.t])
                    tt(kb, "pool", tb_[:], bim[:], snt[:], ALU.mult, [bim.t, trk], [tb_.t])
                    tt(kb, "dve", ta[:], ta[:], tb_[:], ALU.add, [ta.t, tb_.t], [ta.t])
                    tt(kb, "pool", tc_[:], bim[:], cst[:], ALU.mult, [bim.t, trk], [tc_.t])
                    tt(kb, "pool", td[:], bre[:], snt[:], ALU.mult, [bre.t, trk], [td.t])
                    tt(kb, "dve", tc_[:], tc_[:], td[:], ALU.subtract, [tc_.t, td.t], [tc_.t])
                    kb.op("dve", lambda e, q=q, rdec=rdec: e.tensor_tensor_scan(out=gre[:], data0=rdec[:], data1=ta[:], initial=state[:, q, 0:1],
                                                                     op0=ALU.mult, op1=ALU.add), [rdec.t, ta.t, state.t], [gre.t])
                    kb.op("dve", lambda e, q=q, rdec=rdec: e.tensor_tensor_scan(out=gim[:], data0=rdec[:], data1=tc_[:], initial=state[:, q, 1:2],
                                                                     op0=ALU.mult, op1=ALU.add), [rdec.t, tc_.t, state.t], [gim.t])
                    tcopy(kb, "dve", state[:, q, 0:1], gre[:, TB - 1:TB], [gre.t], [state.t])
                    tcopy(kb, "dve", state[:, q, 1:2], gim[:, TB - 1:TB], [gim.t], [state.t])
                    Gq = G[qq % 2]
                    tt(kb, "dve", Gq[:, 0, :], gre[:], cst[:], ALU.mult, [gre.t, trk], [Gq.t])
                    tt(kb, "pool", Gq[:, 1, :], gim[:], snt[:], ALU.mult, [gim.t, trk], [Gq.t])
                    tt(kb, "pool", Gq[:, 2, :], gre[:], snt[:], ALU.mult, [gre.t, trk], [Gq.t])
                    tt(kb, "dve", Gq[:, 3, :], gim[:], cst[:], ALU.mult, [gim.t, trk], [Gq.t])
                    for hb_ in range(2):
                        cs_ = slice(hb_ * 512, (hb_ + 1) * 512)
                        mm(kb, Y[hb_][:, :], LC[:, 0, q, :], Gq[:, 0, cs_], qq == 0, [LC.t, Gq.t], [Y[hb_].t])
                        mm(kb, Y[hb_][:, :], LC[:, 1, q, :], Gq[:, 1, cs_], False, [LC.t, Gq.t], [Y[hb_].t])
                        mm(kb, Y[hb_][:, :], LC[:, 2, q, :], Gq[:, 2, cs_], False, [LC.t, Gq.t], [Y[hb_].t])
                        mm(kb, Y[hb_][:, :], LC[:, 2, q, :], Gq[:, 3, cs_], False, [LC.t, Gq.t], [Y[hb_].t])
                for hb_ in range(2):
                    tc0 = t0 + hb_ * 512
                    stt(kb, yb[:, hb_ * 512:(hb_ + 1) * 512], UTs[:, ch, tc0:tc0 + 512], dcol[:, ch:ch + 1], Y[hb_][:, :], ALU.mult, ALU.add,
                        [UTs.t, dcol.t], [Y[hb_].t, yb.t])
                gelu_tanh(kb, zT[:, ch, t0:t0 + TB], yb[:], y1[:], y2[:], [yb.t], [zT.t], y1.t)
        ocs = [kb.sbuf(f"socs{i}", [128, 2, 512], BF16, es=es) for i in range(2)]
        sg = kb.sbuf("ssg", [128, 512], F32, es=es)
        gi = 0
        for tg in range(8):
            cols = slice(tg * 512, (tg + 1) * 512)
            for oc_ in range(2):
                bank = Y[gi % 2]
                o_st = ocs[gi % 2]
                gi += 1
                for kc in range(2):
                    mm(kb, bank[:, :], Wg[:, kc, oc_ * 128:(oc_ + 1) * 128], zT[:, kc, cols], kc == 0, [Wg.t, zT.t], [bank.t])
                act(kb, sg[:], bank[:, :], AF.Sigmoid, [dcol.t], [bank.t, sg.t], bias=dcol[:, 2 + oc_:3 + oc_])
                tt(kb, "dve", o_st[:, 0, :], zT[:, oc_, cols], sg[:], ALU.mult, [zT.t, sg.t], [o_st.t])
                tt(kb, "pool", o_st[:, 1, :], o_st[:, 0, :], o_st[:, 0, :], ALU.mult, [o_st.t], [o_st.t])
                for sq_ in range(2):
                    kb.dma(C.MIXT[tg * 4:(tg + 1) * 4, :, 6 + 2 * sq_ + oc_, :].rearrange("n p t -> p n t"),
                           o_st[:, sq_, :].rearrange("p (n t) -> p n t", n=4), reads=[o_st.t], writes=[C.MIXT.t])
        kb.barrier()
        kb.flush()


def outproj_phase(kb, C, l):
    P = C.P
    with contextlib.ExitStack() as es:
        Wo = kb.sbuf("Wo", [128, 8, D], BF16, es=es)
        make_stage(kb, C, es, 1024)
        gc = kb.sbuf("ogc", [128, 8], F32, es=es)
        AB = [kb.psum(f"oAB{i}", [128, 512], F32, es=es) for i in range(2)]
        CC = [kb.psum(f"oCC{i}", [128, 512], F32, es=es) for i in range(2)]
        SQ = kb.psum("oSQ", [128, 512], F32, es=es)
        load_cols(kb, C, gc[:, 0:3], gc.t, P["out_norm_nsa"][l], 3, AB[0])
        load_cols(kb, C, gc[:, 3:6], gc.t, P["out_norm_mla"][l], 3, AB[1])
        load_cols(kb, C, gc[:, 6:8], gc.t, P["out_norm_ssm"][l], 2, CC[0])
        for kc in range(8):
            st = stage_load(kb, C, P["w_out"][l, kc * 128:(kc + 1) * 128, :], 128, D)
            cast_scaled(kb, C, Wo[:, kc, :], st[:, 0:D], gc[:, kc:kc + 1], [st.t, gc.t], [Wo.t])
        mts = [kb.sbuf(f"omt{i}", [128, 10, 128], BF16, es=es) for i in range(2)]
        xbs = [kb.sbuf(f"oxb{i}", [128, D], F32, es=es) for i in range(2)]
        xo = [kb.sbuf(f"oxo{i}", [128, D], F32, es=es) for i in range(2)]
        rsc = kb.sbuf("orsc", [128, 4], F32, es=es)
        rscw = kb.sbuf("orscw", [128, 128], F32, es=es)
        xin = C.x_d if l == 0 else C.XR
        for i in range(NT):
            qc = slice(i * 128, (i + 1) * 128)
            mt = mts[i % 2]
            xt = xbs[i % 2]
            xn = xo[i % 2]
            kb.dma(mt[:], C.MIXT[i], reads=[C.MIXT.t], writes=[mt.t])
            kb.dma(xt[:], xin[qc, :], reads=[xin.t], writes=[xt.t])
            for hb_ in range(2):
                cs_ = slice(hb_ * 512, (hb_ + 1) * 512)
                for c in range(6):
                    mm(kb, AB[hb_][:, :], mt[:, c, :], Wo[:, c, cs_], c == 0, [mt.t, Wo.t], [AB[hb_].t])
                for c in range(6, 8):
                    mm(kb, CC[hb_][:, :], mt[:, c, :], Wo[:, c, cs_], c == 6, [mt.t, Wo.t], [CC[hb_].t])
            for c in range(2):
                mm(kb, SQ[:, 0:128], mt[:, 8 + c, :], C.ones[:, 0:128], c == 0, [mt.t, C.ones.t], [SQ.t])
            ts(kb, "dve", rscw[:], SQ[:, 0:128], 1.0 / 256.0, EPS, ALU.mult, ALU.add, [], [SQ.t, rscw.t])
            tcopy(kb, "dve", rsc[:, 0:1], rscw[:, 0:1], [rscw.t], [rsc.t])
            act(kb, rsc[:, 1:2], rsc[:, 0:1], AF.Sqrt, [rsc.t], [rsc.t])
            kb.op("dve", lambda e: e.reciprocal(out=rsc[:, 2:3], in_=rsc[:, 1:2]), [rsc.t], [rsc.t])
            for hb_ in range(2):
                cs_ = slice(hb_ * 512, (hb_ + 1) * 512)
                tt(kb, "dve", xn[:, cs_], AB[hb_][:, :], xt[:, cs_], ALU.add, [xt.t], [AB[hb_].t, xn.t])
                stt(kb, xn[:, cs_], CC[hb_][:, :], rsc[:, 2:3], xn[:, cs_], ALU.mult, ALU.add, [rsc.t], [CC[hb_].t, xn.t])
            kb.dma(C.XR[qc, :], xn[:], reads=[xn.t], writes=[C.XR.t])
        kb.barrier()
        kb.flush()


def ffn_phase(kb, C, l, last):
    P = C.P
    NF = DFF // 128
    TG = 256
    with contextlib.ExitStack() as es:
        make_stage(kb, C, es, 1024)
        Wup = kb.sbuf("Wup", [128, 8, 2 * DFF], BF16, es=es)
        Wdn = kb.sbuf("Wdn", [128, NF, D], BF16, es=es)
        gc = kb.sbuf("fgc", [128, 8], F32, es=es)
        TR = kb.psum("fTR", [128, 1024], BF16, es=es)
        GB = [kb.psum(f"fG{i}", [128, 512], F32, es=es) for i in range(1)]
        VB = [kb.psum(f"fV{i}", [128, 512], F32, es=es) for i in range(1)]
        DB = [[kb.psum(f"fD{t}{h}", [128, 512], F32, es=es) for h in range(2)] for t in range(2)]
        load_cols(kb, C, gc[:], gc.t, P["ffn_norm"][l], 8, DB[0][0])
        cw = kb.sbuf("fcw", [128, 4, NF], F32, es=es)
        for k in range(3):
            load_cols(kb, C, cw[:, k, :], cw.t, P["ffn_conv_w"][l, k], NF, DB[0][1] if k % 2 else DB[1][0])
        load_cols(kb, C, cw[:, 3, :], cw.t, P["ffn_conv_b"][l], NF, DB[1][1])
        for kc in range(8):
            for part in range(6):
                c0 = part * 1024
                w = min(1024, 2 * DFF - c0)
                st = stage_load(kb, C, P["ffn_w_up"][l, kc * 128:(kc + 1) * 128, c0:c0 + w], 128, w)
                cast_scaled(kb, C, Wup[:, kc, c0:c0 + w], st[:, 0:w], gc[:, kc:kc + 1], [st.t, gc.t], [Wup.t])
        for fc in range(NF):
            st = stage_load(kb, C, P["ffn_w_down"][l, fc * 128:(fc + 1) * 128, :], 128, D)
            cast_scaled(kb, C, Wdn[:, fc, :], st[:, 0:D], None, [st.t], [Wdn.t])
        if last:
            fg = kb.sbuf("ffg", [128, D], F32, es=es)
            kb.dma(fg[:], bass.AP(P["final_norm"].h.tensor, 0, [[0, 128], [1, D]]), writes=[fg.t])
        x1s = [kb.sbuf(f"fx1{i}", [128, 2, D], F32, es=es) for i in range(2)]
        xns = [kb.sbuf(f"fxn{i}", [128, D], BF16, es=es) for i in range(2)]
        hT = kb.sbuf("fhT", [128, 8, TG], BF16, es=es)
        aTs = [kb.sbuf(f"faT{i}", [128, TG], BF16, es=es) for i in range(2)]
        halo = kb.sbuf("fhalo", [128, NF, 2], F32, es=es)
        memset(kb, "pool", halo[:], 0.0, [halo.t])
        Gs = [kb.sbuf(f"fGs{i}", [128, TG + 2], F32, es=es) for i in range(2)]
        cv = [kb.sbuf(f"fcv{i}", [128, TG], F32, es=es) for i in range(2)]
        sl = [kb.sbuf(f"fsl{i}", [128, TG], F32, es=es) for i in range(2)]
        it = 0
        for tg in range(S // TG):
            x1 = x1s[tg % 2]
            for tl in range(2):
                tt_ = tg * 2 + tl
                kb.dma(x1[:, tl, :], C.XR[tt_ * 128:(tt_ + 1) * 128, :], reads=[C.XR.t], writes=[x1.t])
                rs = rstd_of(kb, C, x1[:, tl, :], D, [x1.t], None)
                xn = xns[tt_ % 2]
                ts(kb, "dve", xn[:], x1[:, tl, :], rs[:, 0:1], None, ALU.mult, None, [x1.t, rs.t], [xn.t])
                for c in range(8):
                    tr(kb, TR[:, c * 128:(c + 1) * 128], xn[:, c * 128:(c + 1) * 128], C.ident[:], [xn.t, C.ident.t], [TR.t])
                tcopy(kb, "act", hT[:, :, tl * 128:(tl + 1) * 128], TR[:].rearrange("p (c t) -> p c t", c=8), [], [TR.t, hT.t])
            for fc in range(NF):
                gb_ = GB[0]; vb = VB[0]; G_ = Gs[it % 2]; c_ = cv[it % 2]; s_ = sl[it % 2]; aT = aTs[it % 2]
                it += 1
                for kc in range(8):
                    mm(kb, gb_[:, 0:TG], Wup[:, kc, fc * 128:(fc + 1) * 128], hT[:, kc, :], kc == 0, [Wup.t, hT.t], [gb_.t])
                for kc in range(8):
                    mm(kb, vb[:, 0:TG], Wup[:, kc, DFF + fc * 128:DFF + (fc + 1) * 128], hT[:, kc, :], kc == 0, [Wup.t, hT.t], [vb.t])
                tcopy(kb, "pool", G_[:, 0:2], halo[:, fc, :], [halo.t], [G_.t])
                tcopy(kb, "act", G_[:, 2:TG + 2], gb_[:, 0:TG], [], [gb_.t, G_.t])
                tcopy(kb, "pool", halo[:, fc, :], G_[:, TG:TG + 2], [G_.t], [halo.t])
                ts(kb, "dve", c_[:], G_[:, 2:TG + 2], cw[:, 2, fc:fc + 1], cw[:, 3, fc:fc + 1], ALU.mult, ALU.add, [G_.t, cw.t], [c_.t])
                stt(kb, c_[:], G_[:, 1:TG + 1], cw[:, 1, fc:fc + 1], c_[:], ALU.mult, ALU.add, [G_.t, cw.t], [c_.t])
                stt(kb, c_[:], G_[:, 0:TG], cw[:, 0, fc:fc + 1], c_[:], ALU.mult, ALU.add, [G_.t, cw.t], [c_.t])
                act(kb, s_[:], c_[:], AF.Silu, [c_.t], [s_.t])
                tt(kb, "dve", aT[:], vb[:, 0:TG], s_[:], ALU.mult, [s_.t], [vb.t, aT.t])
                for tl in range(2):
                    for hb_ in range(2):
                        mm(kb, DB[tl][hb_][:, :], aT[:, tl * 128:(tl + 1) * 128], Wdn[:, fc, hb_ * 512:(hb_ + 1) * 512], fc == 0,
                           [aT.t, Wdn.t], [DB[tl][hb_].t])
            for tl in range(2):
                tt_ = tg * 2 + tl
                for hb_ in range(2):
                    cs_ = slice(hb_ * 512, (hb_ + 1) * 512)
                    tt(kb, "dve", x1[:, tl, cs_], DB[tl][hb_][:, :], x1[:, tl, cs_], ALU.add, [x1.t], [DB[tl][hb_].t, x1.t])
                if last:
                    rs = rstd_of(kb, C, x1[:, tl, :], D, [x1.t], None)
                    stt(kb, x1[:, tl, :], x1[:, tl, :], rs[:, 0:1], fg[:], ALU.mult, ALU.mult, [x1.t, rs.t, fg.t], [x1.t])
                    kb.dma(C.out_d[tt_ * 128:(tt_ + 1) * 128, :], x1[:, tl, :], reads=[x1.t], writes=[C.out_d.t])
                else:
                    kb.dma(C.XR[tt_ * 128:(tt_ + 1) * 128, :], x1[:, tl, :], reads=[x1.t], writes=[C.XR.t])
        kb.barrier()
        kb.flush()


PARAM_SHAPES = {
    "attn_norm": (4, 1024), "w_in": (4, 1024, 1970), "nsa_pe": (4, 32, 64),
    "nsa_ck_w1": (4, 32, 64, 128), "nsa_ck_b1": (4, 128), "nsa_ck_w2": (4, 128, 64), "nsa_ck_b2": (4, 64),
    "nsa_cv_w1": (4, 32, 64, 128), "nsa_cv_b1": (4, 128), "nsa_cv_w2": (4, 128, 64), "nsa_cv_b2": (4, 64),
    "nsa_gate_b": (4, 18), "mla_q_norm": (4, 384), "mla_kv_norm": (4, 128), "mla_w_uq": (4, 384, 576),
    "mla_w_uk": (4, 128, 384), "mla_w_uv": (4, 128, 384), "ssm_log_dt": (4, 16), "ssm_a_re": (4, 16, 64),
    "ssm_a_im": (4, 16, 64), "ssm_b_re": (4, 16, 64, 16), "ssm_b_im": (4, 16, 64, 16), "ssm_c_re": (4, 16, 16, 64),
    "ssm_c_im": (4, 16, 16, 64), "ssm_d": (4, 16, 16), "ssm_w_glu": (4, 256, 256), "ssm_b_glu": (4, 256),
    "out_norm_nsa": (4, 384), "out_norm_mla": (4, 384), "out_norm_ssm": (4, 256), "w_out": (4, 1024, 1024),
    "ffn_norm": (4, 1024), "ffn_w_up": (4, 1024, 5632), "ffn_conv_w": (4, 3, 2816), "ffn_conv_b": (4, 2816),
    "ffn_w_down": (4, 2816, 1024), "final_norm": (1024,),
}


def build_program(depth=DEPTH, phases=("p1", "nsa", "mla", "ssm", "out", "ffn"), dbg=()):
    nc = bass.Bass("TRN2", target_bir_lowering=False)
    kb = KB(nc)
    C = Ctx()
    C.x_d = kb.dram("x", [S, D], F32, kind="ExternalInput")
    C.pos_d = kb.dram("positions", [1, S], I32, kind="ExternalInput")
    C.invf_d = kb.dram("invf", [96, 1], F32, kind="ExternalInput")
    C.P = {k: kb.dram(k, list(v), F32, kind="ExternalInput") for k, v in PARAM_SHAPES.items()}
    C.out_d = kb.dram("out", [S, D], F32, kind="ExternalOutput")
    C.XR = kb.dram("XR", [S, D], F32)
    C.UT = kb.dram("UT", [256, S], BF16)
    C.LATT = kb.dram("LATT", [512, S], BF16)
    C.KR12 = kb.dram("KR12", [32, S], F32)
    C.MIXT = kb.dram("MIXT", [NT, 128, 10, 128], BF16)
    C.QRD = kb.dram("QRD", [2, 6, 16, S], BF16)
    C.KRD = kb.dram("KRD", [32, S], BF16)
    C.dbg = {}
    setup_consts_pre(kb, C)
    for l in range(depth):
        with contextlib.ExitStack() as esL:
            LS = Ctx()
            LS.QaT = kb.sbuf("QaT", [128, 3, S], BF16, es=esL)
            LS.KcT = kb.sbuf("KcT", [128, S], BF16, es=esL)
            LS.VcT = kb.sbuf("VcT", [128, S], BF16, es=esL)
            LS.KsT = kb.sbuf("KsT", [128, 2, S], BF16, es=esL)
            LS.KwT = kb.sbuf("KwT", [128, 2, S], BF16, es=esL)
            memset(kb, "pool", LS.KsT[:], 0.0, [LS.KsT.t])
            memset(kb, "pool", LS.KwT[:], 0.0, [LS.KwT.t])
            LS.Vs = kb.sbuf("Vs", [128, NT, 2, 66], BF16, es=esL)
            LS.Vw = kb.sbuf("Vw", [128, NT, 2, 66], BF16, es=esL)
            LS.Gt = kb.sbuf("Gt", [128, NT, 18], F32, es=esL)
            memset(kb, "pool", LS.Vs[:], 1.0, [LS.Vs.t])
            memset(kb, "pool", LS.Vw[:], 1.0, [LS.Vw.t])
            if "p1" in phases:
                phase1(kb, C, l, LS)
            if "nsa" in phases:
                nsa_attention(kb, C, l, LS)
            kb.barrier()
            kb.flush()
        if "mla" in phases:
            mla_phase(kb, C, l)
        if "ssm" in phases:
            ssm_phase(kb, C, l)
        if "out" in phases:
            outproj_phase(kb, C, l)
        if "ffn" in phases:
            ffn_phase(kb, C, l, last=(l == depth - 1))
    for name in dbg:
        src = getattr(C, name)
        o = kb.dram("dbg_" + name, list(src.h.shape), src.h.dtype, kind="ExternalOutput")
        kb.dma(o[:], src[:], reads=[src.t], writes=[o.t])
    kb.flush(final=True)
    kb.close()
    return nc


def setup_consts_pre(kb, C):
    hp = kb.sbuf("halfpi", [128, 1], F32)
    memset(kb, "pool", hp[:], math.pi / 2.0, [hp.t])
    C_HALF_PI[0] = hp
    setup_consts(kb, C)
    kb.barrier()
    kb.flush()


_CACHE = {}


def host_consts():
    half = 16
    inv_freq = (10000.0 ** (-np.arange(half, dtype=np.float32) / half)).astype(np.float32)
    return {"invf": np.tile(inv_freq, 6).reshape(96, 1).astype(np.float32)}


def kernel(**inputs):
    if "nc" not in _CACHE:
        _CACHE["nc"] = build_program()
    nc = _CACHE["nc"]
    x = np.ascontiguousarray(inputs["x"], dtype=np.float32)
    pos = np.ascontiguousarray(inputs["positions"], dtype=np.int32)
    shared = {k: np.ascontiguousarray(inputs[k], dtype=np.float32) for k in PARAM_SHAPES}
    shared.update(host_consts())
    in_maps = []
    for b in range(8):
        m = dict(shared)
        m["x"] = x[b]
        m["positions"] = pos[b:b + 1]
        in_maps.append(m)
    res = run_bass_kernel_spmd(nc, in_maps, core_ids=list(range(8)))
    return np.stack([np.asarray(r["out"], dtype=np.float32) for r in res.results], axis=0)
```

```python
import contextlib
import math
import numpy as np
import concourse.bass as bass
import concourse.mybir as mybir
from concourse.bass_utils import run_bass_kernel_spmd

F32 = mybir.dt.float32
BF16 = mybir.dt.bfloat16
I32 = mybir.dt.int32
ALU = mybir.AluOpType
AF = mybir.ActivationFunctionType
AX = mybir.AxisListType

SAME_ENG_SYNC = True
SEM_ROLL = 8000
NDMA = 8

S = 4096
D = 1024
NT = 32
DEPTH = 4
DIN = 1970
DFF = 2816
EPS = 1e-6
NEG = -30000.0
TWO_PI = 2.0 * math.pi
SAFE_2PI = 6.28318


class Trk:
    __slots__ = ("name", "w", "r")

    def __init__(self, name=""):
        self.name = name
        self.w = None
        self.r = {}


class T:
    def __init__(self, h, name, n=0):
        self.h = h
        self.t = Trk(name)
        self.ts = [Trk(f"{name}{i}") for i in range(n)]

    def __getitem__(self, k):
        return self.h[k]


class Op:
    __slots__ = ("eng", "fn", "deps", "sig", "dma", "event", "pseudo")

    def __init__(self, eng, fn, dma):
        self.eng = eng
        self.fn = fn
        self.deps = set()
        self.sig = False
        self.dma = dma
        self.event = None
        self.pseudo = fn is None


class KB:
    def __init__(self, nc):
        self.nc = nc
        self.es = contextlib.ExitStack()
        self.eng = {"pe": nc.tensor, "dve": nc.vector, "act": nc.scalar, "pool": nc.gpsimd, "sp": nc.sync}
        self.ops = []
        self.nsem = 0
        self.nname = 0

    def sbuf(self, name, shape, dtype, n=0, es=None):
        self.nname += 1
        h = (es or self.es).enter_context(self.nc.sbuf_tensor(f"{name}_{self.nname}", list(shape), dtype))
        return T(h, name, n)

    def psum(self, name, shape, dtype, es=None):
        self.nname += 1
        h = (es or self.es).enter_context(self.nc.psum_tensor(f"{name}_{self.nname}", list(shape), dtype))
        return T(h, name)

    def dram(self, name, shape, dtype, kind="Internal", n=0):
        h = self.nc.dram_tensor(name, list(shape), dtype, kind=kind)
        return T(h.ap(), name, n)

    def newsem(self):
        self.nsem += 1
        return self.es.enter_context(self.nc.semaphore(f"s{self.nsem}"))

    def _rec(self, eng, fn, reads, writes, dma):
        idx = len(self.ops)
        o = Op(eng, fn, dma)
        deps = set()
        for t in reads:
            if t.w is not None:
                deps.add(t.w)
        for t in writes:
            if t.w is not None:
                deps.add(t.w)
            deps.update(t.r.values())
        for d in deps:
            od = self.ops[d]
            if (not od.dma) and od.eng == eng and not dma:
                if eng == "pe" or not SAME_ENG_SYNC:
                    continue
            o.deps.add(d)
            od.sig = True
        for t in reads:
            if dma:
                t.r[("dma", idx)] = idx
            else:
                t.r[eng] = idx
        for t in writes:
            t.w = idx
            t.r = {}
        self.ops.append(o)
        return idx

    def op(self, eng, fn, reads=(), writes=()):
        return self._rec(eng, fn, reads, writes, False)

    def dma(self, out, in_, reads=(), writes=(), eng="sp", **kw):
        return self._rec(eng, lambda e: e.dma_start(out=out, in_=in_, **kw), reads, writes, True)

    def barrier(self):
        last = {}
        start = getattr(self, "_bar_start", 0)
        for i, o in enumerate(self.ops):
            if o.pseudo:
                continue
            if o.dma:
                if i >= start:
                    last[("dma", i)] = i
            else:
                last[o.eng] = i
        deps = set(last.values())
        self._bar_start = len(self.ops)
        for e in ("pe", "dve", "act", "pool", "sp"):
            o = Op(e, None, False)
            for d in deps:
                od = self.ops[d]
                if (not od.dma) and od.eng == e and e == "pe":
                    continue
                o.deps.add(d)
                od.sig = True
            self.ops.append(o)

    def _init_emit(self):
        self.sem = {}
        self.cnt = {}
        self.epoch = {}
        self.known = {e: {} for e in self.eng}
        self.dma_slots = {}
        self.dma_i = {}
        self.final_events = {}
        self.emitted = 0
        self._einit = True

    def cur_sem(self, e):
        if e not in self.sem or self.cnt[e] >= SEM_ROLL:
            self.epoch[e] = self.epoch.get(e, -1) + 1
            self.sem[e] = (self.newsem(), (e, self.epoch[e]))
            self.cnt[e] = 0
        return self.sem[e]

    def flush(self, final=False):
        if not getattr(self, "_einit", False):
            self._init_emit()
        nc = self.nc
        known = self.known
        batch = self.ops[self.emitted:]
        lastop = {}
        for o in batch:
            if (not o.dma) and not o.pseudo:
                lastop[o.eng] = o
        for o in lastop.values():
            o.sig = True
        for o in batch:
            E = self.eng[o.eng]
            waits = {}
            for d in o.deps:
                ev = self.ops[d].event
                assert ev is not None, "dep without event"
                h, key, val = ev
                if known[o.eng].get(key, 0) >= val:
                    continue
                if key not in waits or waits[key][1] < val:
                    waits[key] = (h, val)
            slot = None
            if o.dma:
                sl = self.dma_slots.setdefault(o.eng, [None] * NDMA)
                i = self.dma_i.get(o.eng, 0)
                self.dma_i[o.eng] = i + 1
                s = sl[i % NDMA]
                if s is None or s[2] >= SEM_ROLL:
                    if s is not None and known[o.eng].get(s[1], 0) < s[2]:
                        waits[s[1]] = (s[0], s[2])
                    s = [self.newsem(), ("dma", o.eng, i % NDMA, self.nsem), 0]
                    sl[i % NDMA] = s
                elif s[2] > 0 and known[o.eng].get(s[1], 0) < s[2]:
                    if s[1] not in waits or waits[s[1]][1] < s[2]:
                        waits[s[1]] = (s[0], s[2])
                slot = s
            for key, (h, val) in waits.items():
                E.wait_ge(h, val)
                known[o.eng][key] = val
            if o.pseudo:
                continue
            ins = o.fn(E)
            if o.dma:
                slot[2] += 16
                ins.then_inc(slot[0], 16)
                o.event = (slot[0], slot[1], slot[2])
                self.final_events[slot[1]] = (slot[0], slot[2])
            elif o.sig:
                h, key = self.cur_sem(o.eng)
                self.cnt[o.eng] += 1
                ins.then_inc(h, 1)
                o.event = (h, key, self.cnt[o.eng])
        nxt = {}
        for o in reversed(batch):
            if o.dma or o.pseudo:
                continue
            if o.event is None:
                o.event = nxt[o.eng]
            else:
                nxt[o.eng] = o.event
            o.fn = None
        self.emitted = len(self.ops)
        if final:
            for key, (h, val) in self.final_events.items():
                if known["sp"].get(key, 0) < val:
                    nc.sync.wait_ge(h, val)
                    known["sp"][key] = val

    def close(self):
        self.es.close()


def act(kb, out, in_, func, reads, writes, eng="act", **kw):
    kb.op(eng, lambda e: e.activation(out=out, in_=in_, func=func, **kw), reads, writes)


def tcopy(kb, eng, out, in_, reads, writes):
    if eng == "act":
        kb.op("act", lambda e: e.activation(out=out, in_=in_, func=AF.Copy), reads, writes)
    else:
        kb.op(eng, lambda e: e.tensor_copy(out=out, in_=in_), reads, writes)


def tt(kb, eng, out, in0, in1, op, reads, writes):
    kb.op(eng, lambda e: e.tensor_tensor(out=out, in0=in0, in1=in1, op=op), reads, writes)


def ts(kb, eng, out, in0, s1, s2, op0, op1, reads, writes, **kw):
    if op1 is None:
        kb.op(eng, lambda e: e.tensor_scalar(out=out, in0=in0, scalar1=s1, scalar2=None, op0=op0, **kw), reads, writes)
    else:
        kb.op(eng, lambda e: e.tensor_scalar(out=out, in0=in0, scalar1=s1, scalar2=s2, op0=op0, op1=op1, **kw), reads, writes)


def stt(kb, out, in0, scalar, in1, op0, op1, reads, writes):
    kb.op("dve", lambda e: e.scalar_tensor_tensor(out=out, in0=in0, scalar=scalar, in1=in1, op0=op0, op1=op1), reads, writes)


def mm(kb, out, lhsT, rhs, start, reads, writes):
    kb.op("pe", lambda e: e.matmul(out, lhsT=lhsT, rhs=rhs, start=start, stop=True, skip_group_check=True), reads, writes)


def tr(kb, out, in_, ident, reads, writes):
    kb.op("pe", lambda e: e.transpose(out=out, in_=in_, identity=ident), reads, writes)


def memset(kb, eng, ap, val, writes):
    kb.op(eng, lambda e: e.memset(ap, val), [], writes)


def asel(kb, out, in_, pattern, op, fill, base, cm, reads, writes):
    kb.op("pool", lambda e: e.affine_select(out=out, in_=in_, pattern=pattern, compare_op=op, fill=fill, base=base,
                                            channel_multiplier=cm), reads, writes)


def bcast(ap, dims):
    lst = [list(x) for x in ap.ap]
    for pos, cnt in dims:
        lst.insert(pos, [0, cnt])
    return bass.AP(ap.tensor, ap.offset, lst)


class Ctx:
    pass


def make_stage(kb, C, es, cols=2048):
    C.stage = [kb.sbuf(f"stage{i}", [128, cols], F32, es=es) for i in range(2)]


def rstd_of(kb, C, src, n, reads, key):
    i = C.rs_i = getattr(C, "rs_i", 0) + 1
    ssq = C.rs_ssq[i % 4]
    rs = C.rs_out[i % 4]
    junk = C.rs_junk
    act(kb, junk[:, 0:n], src, AF.Square, reads, [junk.t, ssq.t] + ([key] if key is not None else []), accum_out=ssq[:, 0:1])
    ts(kb, "dve", ssq[:, 1:2], ssq[:, 0:1], 1.0 / n, EPS, ALU.mult, ALU.add, [ssq.t], [ssq.t])
    act(kb, ssq[:, 2:3], ssq[:, 1:2], AF.Sqrt, [ssq.t], [ssq.t])
    kb.op("dve", lambda e: e.reciprocal(out=rs[:, 0:1], in_=ssq[:, 2:3]), [ssq.t], [rs.t])
    return rs


def gelu_tanh(kb, out, x, tmp1, tmp2, reads, writes, tmp_trk):
    c = math.sqrt(2.0 / math.pi)
    tt(kb, "dve", tmp1, x, x, ALU.mult, reads, [tmp_trk])
    ts(kb, "dve", tmp1, tmp1, 0.044715, 1.0, ALU.mult, ALU.add, [tmp_trk], [tmp_trk])
    tt(kb, "dve", tmp1, tmp1, x, ALU.mult, reads + [tmp_trk], [tmp_trk])
    act(kb, tmp2, tmp1, AF.Tanh, [tmp_trk], [tmp_trk], scale=c)
    ts(kb, "dve", tmp2, tmp2, 0.5, 0.5, ALU.mult, ALU.add, [tmp_trk], [tmp_trk])
    tt(kb, "dve", out, tmp2, x, ALU.mult, reads + [tmp_trk], writes)


def load_cols(kb, C, dst_ap, dst_trk, src_flat, ncol, bank):
    i = C.lc_i = getattr(C, "lc_i", 0) + 1
    st = C.lc_stage[i % 2]
    npad = 8 if ncol <= 8 else 32
    kb.dma(st[0:ncol, :], src_flat.rearrange("(c p) -> c p", p=128), writes=[st.t])
    tr(kb, bank[:, 0:npad], st[0:npad, :], C.identf[0:npad, 0:npad], [st.t, C.identf.t], [bank.t])
    tmp = C.lc_tmp[i % 2]
    tcopy(kb, "dve", tmp[:, 0:npad], bank[:, 0:npad], [], [bank.t, tmp.t])
    tcopy(kb, "dve", dst_ap, tmp[:, 0:ncol], [tmp.t], [dst_trk])


def stage_load(kb, C, src_ap, rows, cols):
    i = C.st_i = getattr(C, "st_i", 0) + 1
    st = C.stage[i % len(C.stage)]
    kb.dma(st[0:rows, 0:cols], src_ap, writes=[st.t])
    return st


def cast_scaled(kb, C, out, in_, scale_ap, reads, writes):
    i = C.cs_i = getattr(C, "cs_i", 0) + 1
    if scale_ap is None:
        if i % 2:
            tcopy(kb, "pool", out, in_, reads, writes)
        else:
            tcopy(kb, "act", out, in_, reads, writes)
    else:
        if i % 2:
            ts(kb, "pool", out, in_, scale_ap, 0.0, ALU.mult, ALU.add, reads, writes)
        else:
            act(kb, out, in_, AF.Copy, reads, writes, scale=scale_ap)


def setup_consts(kb, C):
    C.ident = kb.sbuf("ident", [128, 128], BF16)
    C.identf = kb.sbuf("identf", [128, 128], F32)
    for t in (C.ident, C.identf):
        memset(kb, "pool", t[:], 0.0, [t.t])
        asel(kb, t[:], t[:], [[-1, 128]], ALU.not_equal, 1.0, 0, 1, [t.t], [t.t])
    C.ones = kb.sbuf("ones", [128, 128], BF16)
    memset(kb, "pool", C.ones[:], 1.0, [C.ones.t])
    C.Ov = kb.sbuf("Ov", [128, 2, 64], BF16)
    memset(kb, "pool", C.Ov[:], 1.0, [C.Ov.t])
    for kt in range(2):
        asel(kb, C.Ov[:, kt, :], C.Ov[:, kt, :], [[-4, 64]], ALU.is_ge, 0.0, 128 * kt + 1, 1, [C.Ov.t], [C.Ov.t])
        asel(kb, C.Ov[:, kt, :], C.Ov[:, kt, :], [[4, 64]], ALU.is_ge, 0.0, 3 - 128 * kt, -1, [C.Ov.t], [C.Ov.t])
    C.rs_ssq = [kb.sbuf(f"rsq{i}", [128, 4], F32) for i in range(4)]
    C.rs_out = [kb.sbuf(f"rso{i}", [128, 1], F32) for i in range(4)]
    C.rs_junk = kb.sbuf("rsjunk", [128, 1024], F32)
    C.lc_stage = [kb.sbuf(f"lcs{i}", [32, 128], F32) for i in range(2)]
    C.lc_tmp = [kb.sbuf(f"lct{i}", [128, 32], F32) for i in range(2)]
    for t in C.lc_stage:
        memset(kb, "pool", t[:], 0.0, [t.t])
    C.invf = kb.sbuf("invf", [96, 1], F32)
    kb.dma(C.invf[:], C.invf_d[:], writes=[C.invf.t])


def sincos_from_turns(kb, x_ap, sin_ap, cos_ap, tmpi_ap, trk, shape_reads):
    tcopy(kb, "dve", tmpi_ap, x_ap, shape_reads + [trk], [trk])
    tt(kb, "dve", x_ap, x_ap, tmpi_ap, ALU.subtract, [trk], [trk])
    act(kb, sin_ap, x_ap, AF.Sin, [trk], [trk], scale=SAFE_2PI)
    act(kb, x_ap, x_ap, AF.Abs, [trk], [trk])
    act(kb, cos_ap, x_ap, AF.Sin, [trk], [trk], scale=-SAFE_2PI, bias=C_HALF_PI[0][0:x_ap.shape[0], 0:1])


C_HALF_PI = [None]


def phase1(kb, C, l, LS):
    P = C.P
    with contextlib.ExitStack() as es:
        make_stage(kb, C, es)
        Win = kb.sbuf("Win", [128, 8, DIN], BF16, es=es)
        Wtok = kb.sbuf("Wtok", [128, 8, 786], BF16, es=es)
        gcol = kb.sbuf("gcol", [128, 8], F32, es=es)
        TR = kb.psum("p1TR", [128, 1024], BF16, es=es)
        TR2 = kb.psum("p1TR2", [128, 1024], BF16, es=es)
        tkA = kb.psum("p1tkA", [128, 512], F32, es=es)
        tkB = kb.psum("p1tkB", [128, 512], F32, es=es)
        FM = [kb.psum(f"p1FM{i}", [128, 512], F32, es=es) for i in range(3)]
        load_cols(kb, C, gcol[:], gcol.t, P["attn_norm"][l], 8, FM[0])
        gb = kb.sbuf("gateb", [128, 18], F32, es=es)
        kb.dma(gb[:], bass.AP(P["nsa_gate_b"].h.tensor, l * 18, [[0, 128], [1, 18]]), writes=[gb.t])
        for kc in range(8):
            st = stage_load(kb, C, P["w_in"][l, kc * 128:(kc + 1) * 128, :], 128, DIN)
            sc = gcol[:, kc:kc + 1]
            cast_scaled(kb, C, Win[:, kc, 0:384].rearrange("p (b a c) -> p a b c", b=3, a=2),
                        st[:, 0:384].rearrange("p (a b c) -> p a b c", a=2, b=3), sc, [st.t, gcol.t], [Win.t])
            cast_scaled(kb, C, Win[:, kc, 384:DIN], st[:, 384:DIN], sc, [st.t, gcol.t], [Win.t])
            cast_scaled(kb, C, Wtok[:, kc, 0:512], st[:, 1170:1682], sc, [st.t, gcol.t], [Wtok.t])
            cast_scaled(kb, C, Wtok[:, kc, 512:640], st[:, 768:896], sc, [st.t, gcol.t], [Wtok.t])
            cast_scaled(kb, C, Wtok[:, kc, 640:786], st[:, 1024:1170], sc, [st.t, gcol.t], [Wtok.t])
        hTs = [kb.sbuf(f"hT{i}", [128, 8, 512], BF16, es=es) for i in range(2)]
        xbs = [kb.sbuf(f"xb{i}", [128, D], F32, es=es) for i in range(2)]
        xns = [kb.sbuf(f"xn{i}", [128, D], BF16, es=es) for i in range(2)]
        lats = [kb.sbuf(f"lat{i}", [128, 512], BF16, es=es) for i in range(2)]
        latTs = [kb.sbuf(f"latT{i}", [128, 4, 128], BF16, es=es) for i in range(2)]
        gtmp = kb.sbuf("gtmp", [128, 18], F32, es=es)
        ust = [kb.sbuf(f"ust{i}", [128, 2, 512], BF16, es=es) for i in range(1)]
        krs = [kb.sbuf(f"krs{i}", [16, 2, 512], F32, es=es) for i in range(1)]
        xin = C.x_d if l == 0 else C.XR
        fmi = 0
        for tg in range(8):
            hT = hTs[tg % 2]
            for tl in range(4):
                tt_ = tg * 4 + tl
                xt = xbs[tt_ % 2]
                xn = xns[tt_ % 2]
                kb.dma(xt[:], xin[tt_ * 128:(tt_ + 1) * 128, :], reads=[xin.t], writes=[xt.t])
                rs = rstd_of(kb, C, xt[:], D, [xt.t], None)
                ts(kb, "dve", xn[:], xt[:], rs[:, 0:1], None, ALU.mult, None, [xt.t, rs.t], [xn.t])
                for c in range(8):
                    tr(kb, TR[:, c * 128:(c + 1) * 128], xn[:, c * 128:(c + 1) * 128], C.ident[:], [xn.t, C.ident.t], [TR.t])
                tcopy(kb, "act", hT[:, :, tl * 128:(tl + 1) * 128], TR[:].rearrange("p (c t) -> p c t", c=8), [], [TR.t, hT.t])
                for kc in range(8):
                    mm(kb, tkA[:, 0:512], hT[:, kc, tl * 128:(tl + 1) * 128], Wtok[:, kc, 0:512], kc == 0, [hT.t, Wtok.t], [tkA.t])
                for kc in range(8):
                    mm(kb, tkB[:, 0:274], hT[:, kc, tl * 128:(tl + 1) * 128], Wtok[:, kc, 512:786], kc == 0, [hT.t, Wtok.t], [tkB.t])
                tcopy(kb, "act", LS.Vs[:, tt_, :, 0:64], tkB[:, 0:128].rearrange("p (k d) -> p k d", k=2), [], [tkB.t, LS.Vs.t])
                tcopy(kb, "dve", LS.Vw[:, tt_, :, 0:64], tkB[:, 128:256].rearrange("p (k d) -> p k d", k=2), [], [tkB.t, LS.Vw.t])
                tt(kb, "dve", gtmp[:], tkB[:, 256:274], gb[:], ALU.add, [gb.t], [tkB.t, gtmp.t])
                act(kb, LS.Gt[:, tt_, :], gtmp[:], AF.Sigmoid, [gtmp.t], [LS.Gt.t])
                lat = lats[tt_ % 2]
                latT = latTs[tt_ % 2]
                rq = rstd_of(kb, C, tkA[:, 0:384], 384, [], tkA.t)
                ts(kb, "dve", lat[:, 0:384], tkA[:, 0:384], rq[:, 0:1], None, ALU.mult, None, [rq.t], [tkA.t, lat.t])
                rk = rstd_of(kb, C, tkA[:, 384:512], 128, [], tkA.t)
                ts(kb, "dve", lat[:, 384:512], tkA[:, 384:512], rk[:, 0:1], None, ALU.mult, None, [rk.t], [tkA.t, lat.t])
                for c in range(4):
                    tr(kb, TR2[:, c * 128:(c + 1) * 128], lat[:, c * 128:(c + 1) * 128], C.ident[:], [lat.t, C.ident.t], [TR2.t])
                tcopy(kb, "act", latT[:], TR2[:, 0:512].rearrange("p (c t) -> p c t", c=4), [], [TR2.t, latT.t])
                kb.dma(C.LATT[:, tt_ * 128:(tt_ + 1) * 128].rearrange("(c p) t -> p c t", p=128), latT[:], reads=[latT.t], writes=[C.LATT.t])
            cols = slice(tg * 512, (tg + 1) * 512)
            specs = [(0, 128, LS.QaT[:, 0, cols], LS.QaT.t), (128, 128, LS.QaT[:, 1, cols], LS.QaT.t), (256, 128, LS.QaT[:, 2, cols], LS.QaT.t),
                     (384, 128, LS.KcT[:, cols], LS.KcT.t), (512, 128, LS.VcT[:, cols], LS.VcT.t),
                     (640, 128, LS.KsT, LS.KsT.t), (896, 128, LS.KwT, LS.KwT.t)]
            u_st = ust[0]
            kr_st = krs[0]
            specs += [(1714, 128, u_st[:, 0, :], u_st.t), (1842, 128, u_st[:, 1, :], u_st.t),
                      (1682, 16, kr_st[:, 0, :], kr_st.t), (1698, 16, kr_st[:, 1, :], kr_st.t)]
            for (c0, w, dst, dtrk) in specs:
                bank = FM[fmi % 3]
                fmi += 1
                for kc in range(8):
                    mm(kb, bank[0:w, :], Win[:, kc, c0:c0 + w], hT[:, kc, :], kc == 0, [Win.t, hT.t], [bank.t])
                if dst is LS.KsT or dst is LS.KwT:
                    tcopy(kb, "act", dst[0:64, 0, cols], bank[0:64, :], [], [bank.t, dtrk])
                    tcopy(kb, "dve", dst[64:128, 1, cols], bank[64:128, :], [], [bank.t, dtrk])
                else:
                    tcopy(kb, "act" if fmi % 2 else "dve", dst, bank[0:w, :], [], [bank.t, dtrk])
            kb.dma(C.UT[:, cols].rearrange("(c p) t -> p c t", p=128), u_st[:], reads=[u_st.t], writes=[C.UT.t])
            kb.dma(C.KR12[:, cols].rearrange("(two r) t -> r two t", two=2), kr_st[:], reads=[kr_st.t], writes=[C.KR12.t])
        kb.barrier()
        kb.flush()


def nsa_compress(kb, C, l, LS, es):
    import os
    steps = os.environ.get("CMP_STEPS", "init,pe,w,dma4,gath,mm1,mmpe,gelu,mm2k,mm2v").split(",")
    P = C.P
    KcmpT = kb.sbuf("KcmpT", [128, 2, 256], BF16, es=es)
    Vcmp = kb.sbuf("Vcmp", [128, 2, 2, 66], BF16, es=es)
    if "init" in steps:
        memset(kb, "pool", KcmpT[:], 0.0, [KcmpT.t])
        memset(kb, "pool", Vcmp[:], 0.0, [Vcmp.t])
        memset(kb, "pool", Vcmp[:, :, :, 64:65], 1.0, [Vcmp.t])
    with contextlib.ExitStack() as es2:
        make_stage(kb, C, es2, 1024)
        W1 = kb.sbuf("W1", [128, 32, 128], BF16, es=es2)
        W2 = kb.sbuf("W2", [128, 128], BF16, es=es2)
        peT = kb.sbuf("peT", [128, 32], BF16, es=es2)
        gath = kb.sbuf("gath", [128, 32, 256], BF16, es=es2)
        b2bc = kb.sbuf("b2bc", [128, 64], F32, es=es2)
        pe_st = kb.sbuf("pe_st", [32, 64], F32, es=es2)
        pe_b = kb.sbuf("pe_b", [32, 128], BF16, es=es2)
        hb = kb.sbuf("hb", [128, 2], F32, es=es2)
        b2c = kb.sbuf("b2c", [128, 1], F32, es=es2)
        hx = kb.sbuf("hx", [128, 256], F32, es=es2)
        ht1 = kb.sbuf("ht1", [128, 256], F32, es=es2)
        ht2 = kb.sbuf("ht2", [128, 256], F32, es=es2)
        hidT = kb.sbuf("hidT", [128, 256], BF16, es=es2)
        HB = kb.psum("cmpH", [128, 512], F32, es=es2)
        OB = kb.psum("cmpO", [128, 512], F32, es=es2)
        TRb = kb.psum("cmpT", [128, 1024], BF16, es=es2)
        memset(kb, "pool", gath[:], 0.0, [gath.t])
        if "pe" in steps:
            kb.dma(pe_st[:], P["nsa_pe"][l], writes=[pe_st.t])
            tcopy(kb, "dve", pe_b[:, 0:64], pe_st[:], [pe_st.t], [pe_b.t])
            tcopy(kb, "dve", pe_b[:, 64:128], pe_st[:], [pe_st.t], [pe_b.t])
            tr(kb, TRb[:, 0:32], pe_b[:], C.ident[0:32, 0:32], [pe_b.t, C.ident.t], [TRb.t])
            tcopy(kb, "dve", peT[:], TRb[:, 0:32], [], [TRb.t, peT.t])
            memset(kb, "pool", hidT[:], 0.0, [hidT.t])
        for which in ("k", "v"):
            w1n, b1n, w2n, b2n = (f"nsa_c{which}_w1", f"nsa_c{which}_b1", f"nsa_c{which}_w2", f"nsa_c{which}_b2")
            src = LS.KcT if which == "k" else LS.VcT
            if "w" in steps:
                for half in range(2):
                    for lq in range(4):
                        i_ = C.st_i = getattr(C, "st_i", 0) + 1
                        st = C.stage[i_ % len(C.stage)]
                        hp_ = slice(half * 64, (half + 1) * 64)
                        kb.dma(st[hp_, 0:1024].rearrange("p (l f) -> p l f", l=8), P[w1n][l, lq * 8:(lq + 1) * 8].rearrange("l d f -> d l f"), writes=[st.t])
                        cast_scaled(kb, C, W1[hp_, lq * 8:(lq + 1) * 8, :],
                                    st[hp_, 0:1024].rearrange("p (l f) -> p l f", l=8), None, [st.t], [W1.t])
                st = stage_load(kb, C, P[w2n][l], 128, 64)
                cast_scaled(kb, C, W2[:, 0:64], st[:, 0:64], None, [st.t], [W2.t])
                cast_scaled(kb, C, W2[:, 64:128], st[:, 0:64], None, [st.t], [W2.t])
            if "dma4" in steps:
                kb.dma(hb[:, 0:1], P[b1n][l].rearrange("(p o) -> p o", o=1), writes=[hb.t])
                kb.dma(b2c[0:64, :], P[b2n][l].rearrange("(p o) -> p o", o=1), writes=[b2c.t])
                kb.dma(b2c[64:128, :], P[b2n][l].rearrange("(p o) -> p o", o=1), writes=[b2c.t])
                kb.dma(b2bc[:], bass.AP(P[b2n].h.tensor, l * 64, [[0, 128], [1, 64]]), writes=[b2bc.t])
            if "gath" in steps:
                for lq in range(32):
                    srcv = bass.AP(src.h, src[:, lq:lq + 1].offset, [list(src[:, 0:1].ap[0]), [16, 255]])
                    tcopy(kb, ("dve", "pool", "act")[lq % 3], gath[:, lq, 0:255], srcv, [src.t], [gath.t])
                tcopy(kb, "dve", gath[:, :, 255], peT[:], [peT.t], [gath.t])
            for kv in range(2):
                lo, hi = kv * 64, (kv + 1) * 64
                first = True
                if "mm1" in steps:
                    for lq in range(32):
                        mm(kb, HB[:, 0:256], W1[lo:hi, lq, :], gath[lo:hi, lq, 0:256], first, [W1.t, gath.t], [HB.t])
                        first = False
                if "mm1" in steps:
                    tcopy(kb, "act", hx[:, 0:256], HB[:, 0:256], [], [HB.t, hx.t])
                    ts(kb, "dve", hx[:, 0:255], hx[:, 0:255], hx[:, 255:256], hb[:, 0:1], ALU.add, ALU.add, [hx.t, hb.t], [hx.t])
                if "gelu" in steps:
                    gelu_tanh(kb, hidT[:, 0:255], hx[:, 0:255], ht1[:, 0:255], ht2[:, 0:255], [hx.t], [hidT.t], ht1.t)
                if which == "k":
                    if "mm2k" in steps:
                        mm(kb, OB[:, 0:256], W2[:], hidT[:, 0:256], True, [W2.t, hidT.t], [OB.t])
                        ts(kb, "dve", KcmpT[lo:hi, kv, 0:255], OB[lo:hi, 0:255], b2c[lo:hi, 0:1], None, ALU.add, None, [b2c.t], [OB.t, KcmpT.t])
                elif "mm2v" in steps:
                    for kt in range(2):
                        n = 128
                        mm(kb, OB[0:n, kt * 64:(kt + 1) * 64], hidT[:, kt * 128:kt * 128 + n], W2[:, 0:64], kt == 0, [W2.t, hidT.t], [OB.t])
                    for kt in range(2):
                        n = 128
                        tt(kb, "dve", Vcmp[0:n, kt, kv, 0:64], OB[0:n, kt * 64:(kt + 1) * 64], b2bc[0:n, :], ALU.add, [b2bc.t], [OB.t, Vcmp.t])
        kb.barrier()
        kb.flush()
    return KcmpT, Vcmp


def nsa_attention(kb, C, l, LS):
    P = C.P
    with contextlib.ExitStack() as es:
        KcmpT, Vcmp = nsa_compress(kb, C, l, LS, es)
        import os
        if os.environ.get("NSA_COMPRESS_ONLY"):
            kb.barrier()
            kb.flush()
            return
        C.Eall = kb.sbuf("Eall", [128, S], BF16, es=es)
        memset(kb, "pool", C.Eall[:], 1.0, [C.Eall.t])
        asel(kb, C.Eall[:], C.Eall[:], [[1, S]], ALU.is_ge, 0.0, 0, -64, [C.Eall.t], [C.Eall.t])
        asel(kb, C.Eall[:], C.Eall[:], [[-1, S]], ALU.is_ge, 0.0, 63, 64, [C.Eall.t], [C.Eall.t])
        SC = [kb.psum(f"nsSC{i}", [128, 512], F32, es=es) for i in range(2)]
        OC = kb.psum("nsOC", [128, 512], F32, es=es)
        OS = kb.psum("nsOS", [128, 512], F32, es=es)
        OW = kb.psum("nsOW", [128, 512], F32, es=es)
        IM = kb.psum("nsIM", [128, 512], F32, es=es)
        TRb = kb.psum("nsTR", [128, 1024], BF16, es=es)
        PTs = [kb.sbuf(f"PT{i}", [128, 384], BF16, es=es) for i in range(4)]
        oc = kb.sbuf("oc", [128, 6, 66], F32, es=es)
        osb = kb.sbuf("osb", [128, 6, 66], F32, es=es)
        owb = kb.sbuf("owb", [128, 6, 66], F32, es=es)
        rc = kb.sbuf("rc", [128, 3, 6], F32, es=es)
        imp = kb.sbuf("imp", [128, 2, 64], F32, es=es)
        imp2 = kb.sbuf("imp2", [128, 2, 64], F32, es=es)
        m8 = kb.sbuf("m8", [128, 2, 16], F32, es=es)
        negm = kb.sbuf("negm", [128, 2, 64], BF16, es=es)
        negmT = kb.sbuf("negmT", [128, 2, 3, 128], BF16, es=es)
        memset(kb, "pool", negmT[:], 0.0, [negmT.t])
        oa = kb.sbuf("oa", [128, 6, 64], F32, es=es)
        otmp = kb.sbuf("otmp", [128, 6, 64], F32, es=es)
        oab = kb.sbuf("oab", [128, 384], BF16, es=es)
        oaTs = [kb.sbuf(f"oaT{i}", [128, 3, 128], BF16, es=es) for i in range(2)]
        pti = [0]
        sci = [0]

        pending = [None]

        def flush_pending():
            if pending[0] is not None:
                f = pending[0]
                pending[0] = None
                f()

        def unit(score_fn, ncols, scale, mask, pv_fn):
            bank = SC[sci[0] % 2]
            sci[0] += 1
            PT = PTs[pti[0] % 4]
            pti[0] += 1
            score_fn(bank)
            flush_pending()
            act(kb, PT[:, 0:ncols], bank[:, 0:ncols], AF.Exp, [], [bank.t, PT.t], scale=scale)
            if mask is not None:
                base, cm, step = mask
                asel(kb, PT[:, 0:ncols].rearrange("p (g q) -> p g q", q=128), PT[:, 0:ncols].rearrange("p (g q) -> p g q", q=128),
                     [[0, ncols // 128], [step, 128]], ALU.is_ge, 0.0, base, cm, [PT.t], [PT.t])
            pending[0] = lambda: pv_fn(PT)

        for i in range(NT):
            qc = slice(i * 128, (i + 1) * 128)
            nkt = 1 if (8 * i + 6) < 128 else 2
            firstC = [True]
            firstI = [True]
            for kv in range(2):
                lo, hi = kv * 64, (kv + 1) * 64
                for kt in range(nkt):
                    def score_fn(bank, kt=kt, lo=lo, hi=hi, kv=kv):
                        mm(kb, bank[:, 0:384], KcmpT[:, kv, kt * 128:(kt + 1) * 128], LS.QaT[:, :, qc], True, [KcmpT.t, LS.QaT.t], [bank.t])

                    def pv_fn(PT, kt=kt, kv=kv):
                        for g in range(3):
                            h = kv * 3 + g
                            mm(kb, OC[:, h * 80:h * 80 + 66], PT[:, g * 128:(g + 1) * 128], Vcmp[:, kt, kv, :], firstC[0], [PT.t, Vcmp.t], [OC.t])
                            firstC[0] = False
                            mm(kb, IM[:, h * 64:(h + 1) * 64], PT[:, g * 128:(g + 1) * 128], C.Ov[:, kt, :], firstI[0], [PT.t, C.Ov.t], [IM.t])
                            firstI[0] = False
                    unit(score_fn, 384, 0.125, (128 * i - 2048 * kt - 31, -16, 1), pv_fn)
            flush_pending()
            tcopy(kb, "act", oc[:], OC[:, 0:480].rearrange("p (h d) -> p h d", h=6)[:, :, 0:66], [], [OC.t, oc.t])
            ts(kb, "dve", rc[:, 0, :], oc[:, :, 64], 1e-30, None, ALU.max, None, [oc.t], [rc.t])
            kb.op("dve", lambda e: e.reciprocal(out=rc[:, 0, :], in_=rc[:, 0, :]), [rc.t], [rc.t])
            for kv in range(2):
                ts(kb, "dve", imp[:, kv, :], IM[:, (kv * 3) * 64:(kv * 3 + 1) * 64], rc[:, 0, kv * 3:kv * 3 + 1], None, ALU.mult, None, [rc.t], [IM.t, imp.t])
                for g in (1, 2):
                    h = kv * 3 + g
                    stt(kb, imp[:, kv, :], IM[:, h * 64:(h + 1) * 64], rc[:, 0, h:h + 1], imp[:, kv, :], ALU.mult, ALU.add, [rc.t], [IM.t, imp.t])
            for hf in range(2):
                cur = 2 * i + hf
                pr = slice(hf * 64, (hf + 1) * 64)
                if cur + 1 < 64:
                    memset(kb, "pool", imp[pr, :, cur + 1:64], -1.0, [imp.t])
                memset(kb, "pool", imp[pr, :, 0:1], 100.0, [imp.t])
                memset(kb, "pool", imp[pr, :, max(cur - 1, 0):cur + 1], 100.0, [imp.t])
            for kv in range(2):
                kb.op("dve", lambda e, kv=kv: e.max(out=m8[:, kv, 0:8], in_=imp[:, kv, :]), [imp.t], [m8.t])
                kb.op("dve", lambda e, kv=kv: e.match_replace(out=imp2[:, kv, :], in_to_replace=m8[:, kv, 0:8], in_values=imp[:, kv, :], imm_value=-1e30), [imp.t, m8.t], [imp2.t])
                kb.op("dve", lambda e, kv=kv: e.max(out=m8[:, kv, 8:16], in_=imp2[:, kv, :]), [imp2.t], [m8.t])
                ts(kb, "dve", negm[:, kv, :], imp[:, kv, :], m8[:, kv, 15:16], NEG, ALU.is_lt, ALU.mult, [imp.t, m8.t], [negm.t])
                tr(kb, TRb[0:64, kv * 128:(kv + 1) * 128], negm[:, kv, :], C.ident[:], [negm.t, C.ident.t], [TRb.t])
            for g in range(3):
                tcopy(kb, "dve" if g != 1 else "act", negmT[0:64, :, g, :], TRb[0:64, 0:256].rearrange("p (k q) -> p k q", k=2), [], [TRb.t, negmT.t])
            firstS = [True]
            for kv in range(2):
                lo, hi = kv * 64, (kv + 1) * 64
                for j in range(i + 1):
                    kc = slice(j * 128, (j + 1) * 128)

                    def score_fn(bank, kc=kc, lo=lo, hi=hi, kv=kv):
                        mm(kb, bank[:, 0:384], LS.KsT[:, kv, kc], LS.QaT[:, :, qc], True, [LS.KsT.t, LS.QaT.t], [bank.t])
                        mm(kb, bank[:, 0:384], C.Eall[:, kc], negmT[:, kv, :, :], False, [C.Eall.t, negmT.t], [bank.t])

                    def pv_fn(PT, j=j, kv=kv):
                        for g in range(3):
                            h = kv * 3 + g
                            mm(kb, OS[:, h * 80:h * 80 + 66], PT[:, g * 128:(g + 1) * 128], LS.Vs[:, j, kv, :], firstS[0], [PT.t, LS.Vs.t], [OS.t])
                            firstS[0] = False
                    unit(score_fn, 384, 0.125, (0, -1, 1) if j == i else None, pv_fn)
            firstW = [True]
            for kv in range(2):
                lo, hi = kv * 64, (kv + 1) * 64
                for j in range(max(0, i - 4), i + 1):
                    kc = slice(j * 128, (j + 1) * 128)

                    def score_fn(bank, kc=kc, lo=lo, hi=hi, kv=kv):
                        mm(kb, bank[:, 0:384], LS.KwT[:, kv, kc], LS.QaT[:, :, qc], True, [LS.KwT.t, LS.QaT.t], [bank.t])

                    def pv_fn(PT, j=j, kv=kv):
                        for g in range(3):
                            h = kv * 3 + g
                            mm(kb, OW[:, h * 80:h * 80 + 66], PT[:, g * 128:(g + 1) * 128], LS.Vw[:, j, kv, :], firstW[0], [PT.t, LS.Vw.t], [OW.t])
                            firstW[0] = False
                    mask = None
                    if j == i:
                        mask = (0, -1, 1)
                    elif j == i - 4:
                        mask = (-1, 1, -1)
                    unit(score_fn, 384, 0.125, mask, pv_fn)
            flush_pending()
            tcopy(kb, "act", osb[:], OS[:, 0:480].rearrange("p (h d) -> p h d", h=6)[:, :, 0:66], [], [OS.t, osb.t])
            tcopy(kb, "act", owb[:], OW[:, 0:480].rearrange("p (h d) -> p h d", h=6)[:, :, 0:66], [], [OW.t, owb.t])
            for bi, ob in ((1, osb), (2, owb)):
                ts(kb, "dve", rc[:, bi, :], ob[:, :, 64], 1e-30, None, ALU.max, None, [ob.t], [rc.t])
                kb.op("dve", lambda e, bi=bi: e.reciprocal(out=rc[:, bi, :], in_=rc[:, bi, :]), [rc.t], [rc.t])
            tt(kb, "dve", rc[:], rc[:], LS.Gt[:, i, :].rearrange("p (h b) -> p b h", b=3), ALU.mult, [rc.t, LS.Gt.t], [rc.t])
            tt(kb, "dve", oa[:], oc[:, :, 0:64], bcast(rc[:, 0, :], [(2, 64)]), ALU.mult, [oc.t, rc.t], [oa.t])
            for bi, ob in ((1, osb), (2, owb)):
                tt(kb, "dve", otmp[:], ob[:, :, 0:64], bcast(rc[:, bi, :], [(2, 64)]), ALU.mult, [ob.t, rc.t], [otmp.t])
                tt(kb, "dve", oa[:], oa[:], otmp[:], ALU.add, [oa.t, otmp.t], [oa.t])
            ra = rstd_of(kb, C, oa[:].rearrange("p h d -> p (h d)"), 384, [oa.t], None)
            ts(kb, "dve", oab[:], oa[:].rearrange("p h d -> p (h d)"), ra[:, 0:1], None, ALU.mult, None, [oa.t, ra.t], [oab.t])
            oaT = oaTs[i % 2]
            for c in range(3):
                tr(kb, TRb[:, 256 + c * 128:256 + (c + 1) * 128], oab[:, c * 128:(c + 1) * 128], C.ident[:], [oab.t, C.ident.t], [TRb.t])
            tcopy(kb, "act", oaT[:], TRb[:, 256:640].rearrange("p (c t) -> p c t", c=3), [], [TRb.t, oaT.t])
            kb.dma(C.MIXT[i, :, 0:3, :], oaT[:], reads=[oaT.t], writes=[C.MIXT.t])
        kb.barrier()
        kb.flush()


def mla_phase(kb, C, l):
    P = C.P
    sc = 96.0 ** -0.5
    with contextlib.ExitStack() as es:
        KnT = kb.sbuf("KnT", [128, 6, S], BF16, es=es)
        memset(kb, "pool", KnT[:], 0.0, [KnT.t])
        QnT = kb.sbuf("QnT", [128, 3, S], BF16, es=es)
        KrT = kb.sbuf("KrT", [128, S], BF16, es=es)
        memset(kb, "pool", KrT[:], 0.0, [KrT.t])
        Vm = kb.sbuf("Vm", [128, NT, 6, 66], BF16, es=es)
        memset(kb, "pool", Vm[:], 1.0, [Vm.t])
        with contextlib.ExitStack() as es2:
            make_stage(kb, C, es2, 1024)
            posf = kb.sbuf("posf", [96, S], F32, es=es2)
            posi = kb.sbuf("posi", [96, S], I32, es=es2)
            kb.dma(posi[:], bass.AP(C.pos_d.h.tensor, 0, [[0, 96], [1, S]]), writes=[posi.t])
            tcopy(kb, "dve", posf[:], posi[:], [posi.t], [posf.t])
            ts(kb, "dve", posf[:], posf[:], C.invf[:, 0:1], 1.0 / TWO_PI, ALU.mult, ALU.mult, [posf.t, C.invf.t], [posf.t])
            C.posf = posf
            Wq = kb.sbuf("Wq", [128, 3, 576], BF16, es=es2)
            Wk = kb.sbuf("Wk", [128, 384], BF16, es=es2)
            Wv = kb.sbuf("Wv", [128, 384], BF16, es=es2)
            qg = kb.sbuf("qg", [128, 4], F32, es=es2)
            PB = [kb.psum(f"mlP{i}", [128, 512], F32, es=es2) for i in range(4)]
            load_cols(kb, C, qg[:, 0:3], qg.t, P["mla_q_norm"][l], 3, PB[0])
            kb.dma(qg[:, 3:4], P["mla_kv_norm"][l].rearrange("(p o) -> p o", o=1), writes=[qg.t])
            for kc in range(3):
                st = stage_load(kb, C, P["mla_w_uq"][l, kc * 128:(kc + 1) * 128, :], 128, 576)
                sv = st[:, 0:576].rearrange("p (h e) -> p h e", h=6)
                g = qg[:, kc:kc + 1]
                cast_scaled(kb, C, Wq[:, kc, 0:384].rearrange("p (h e) -> p h e", h=6), sv[:, :, 0:64], g, [st.t, qg.t], [Wq.t])
                cast_scaled(kb, C, Wq[:, kc, 384:480].rearrange("p (h e) -> p h e", h=6), sv[:, :, 64:80], g, [st.t, qg.t], [Wq.t])
                cast_scaled(kb, C, Wq[:, kc, 480:576].rearrange("p (h e) -> p h e", h=6), sv[:, :, 80:96], g, [st.t, qg.t], [Wq.t])
            st = stage_load(kb, C, P["mla_w_uk"][l], 128, 384)
            cast_scaled(kb, C, Wk[:], st[:, 0:384], qg[:, 3:4], [st.t, qg.t], [Wk.t])
            st = stage_load(kb, C, P["mla_w_uv"][l], 128, 384)
            cast_scaled(kb, C, Wv[:], st[:, 0:384], qg[:, 3:4], [st.t, qg.t], [Wv.t])
            lat = [kb.sbuf(f"mlat{i}", [128, 4, 512], BF16, es=es2) for i in range(1)]
            kr_raw = [kb.sbuf(f"krr{i}", [16, 2, 512], F32, es=es2) for i in range(1)]
            tb = [kb.sbuf(f"mtb{i}", [96, 512], F32, es=es2) for i in range(2)]
            tbi = kb.sbuf("mtbi", [96, 512], I32, es=es2)
            sn = [kb.sbuf(f"msn{i}", [96, 512], F32, es=es2) for i in range(2)]
            cs = [kb.sbuf(f"mcs{i}", [96, 512], F32, es=es2) for i in range(2)]
            x1 = kb.sbuf("mx1", [96, 512], F32, es=es2)
            x2 = kb.sbuf("mx2", [96, 512], F32, es=es2)
            r1 = kb.sbuf("mr1", [96, 512], F32, es=es2)
            r2 = kb.sbuf("mr2", [96, 512], F32, es=es2)
            qo = [kb.sbuf(f"mqo{i}", [96, 2, 512], BF16, es=es2) for i in range(2)]
            ko = [kb.sbuf(f"mko{i}", [16, 2, 512], BF16, es=es2) for i in range(2)]
            pbi = 0
            for tg in range(8):
                cols = slice(tg * 512, (tg + 1) * 512)
                la = lat[0]
                kb.dma(la[:], C.LATT[:, cols].rearrange("(c p) t -> p c t", p=128), reads=[C.LATT.t], writes=[la.t])
                kr = kr_raw[0]
                kb.dma(kr[:], C.KR12[:, cols].rearrange("(two r) t -> r two t", two=2), reads=[C.KR12.t], writes=[kr.t])
                tbt = tb[tg % 2]
                snt = sn[tg % 2]
                cst = cs[tg % 2]
                tcopy(kb, "pool", tbt[:], C.posf[:, cols], [C.posf.t], [tbt.t])
                sincos_from_turns(kb, tbt[:], snt[:], cst[:], tbi[:], tbt.t, [])
                trk = tbt.t
                for c in range(3):
                    bank = PB[pbi % 4]; pbi += 1
                    mm(kb, bank[:, :], Wk[:, c * 128:(c + 1) * 128], la[:, 3, :], True, [Wk.t, la.t], [bank.t])
                    tcopy(kb, "act", KnT[0:64, 2 * c, cols], bank[0:64, :], [], [bank.t, KnT.t])
                    tcopy(kb, "dve", KnT[64:128, 2 * c + 1, cols], bank[64:128, :], [], [bank.t, KnT.t])
                for c in range(3):
                    bank = PB[pbi % 4]; pbi += 1
                    for kc in range(3):
                        mm(kb, bank[:, :], Wq[:, kc, c * 128:(c + 1) * 128], la[:, kc, :], kc == 0, [Wq.t, la.t], [bank.t])
                    tcopy(kb, "dve" if c % 2 else "act", QnT[:, c, cols], bank[:, :], [], [bank.t, QnT.t])
                for tl in range(4):
                    bank = PB[pbi % 4]; pbi += 1
                    mm(kb, bank[:, 0:384], la[:, 3, tl * 128:(tl + 1) * 128], Wv[:], True, [Wv.t, la.t], [bank.t])
                    tcopy(kb, "act", Vm[:, tg * 4 + tl, :, 0:64], bank[:, 0:384].rearrange("p (h d) -> p h d", h=6), [], [bank.t, Vm.t])
                b1 = PB[pbi % 4]; pbi += 1
                b2 = PB[pbi % 4]; pbi += 1
                for kc in range(3):
                    mm(kb, b1[0:96, :], Wq[:, kc, 384:480], la[:, kc, :], kc == 0, [Wq.t, la.t], [b1.t])
                for kc in range(3):
                    mm(kb, b2[0:96, :], Wq[:, kc, 480:576], la[:, kc, :], kc == 0, [Wq.t, la.t], [b2.t])
                tcopy(kb, "act", x1[:], b1[0:96, :], [], [b1.t, x1.t])
                tcopy(kb, "act", x2[:], b2[0:96, :], [], [b2.t, x2.t])
                q_o = qo[tg % 2]
                tt(kb, "dve", r1[:], x1[:], cst[:], ALU.mult, [x1.t, trk], [r1.t])
                tt(kb, "pool", r2[:], x2[:], snt[:], ALU.mult, [x2.t, trk], [r2.t])
                tt(kb, "dve", q_o[:, 0, :], r1[:], r2[:], ALU.subtract, [r1.t, r2.t], [q_o.t])
                tt(kb, "dve", r1[:], x2[:], cst[:], ALU.mult, [x2.t, trk], [r1.t])
                tt(kb, "pool", r2[:], x1[:], snt[:], ALU.mult, [x1.t, trk], [r2.t])
                tt(kb, "dve", q_o[:, 1, :], r1[:], r2[:], ALU.add, [r1.t, r2.t], [q_o.t])
                for two in range(2):
                    kb.dma(C.QRD[two, :, :, cols].rearrange("h r t -> (h r) t"), q_o[:, two, :], reads=[q_o.t], writes=[C.QRD.t])
                k_o = ko[tg % 2]
                tt(kb, "dve", r1[0:16, :], kr[:, 0, :], cst[0:16, :], ALU.mult, [kr.t, trk], [r1.t])
                tt(kb, "pool", r2[0:16, :], kr[:, 1, :], snt[0:16, :], ALU.mult, [kr.t, trk], [r2.t])
                tt(kb, "dve", k_o[:, 0, :], r1[0:16, :], r2[0:16, :], ALU.subtract, [r1.t, r2.t], [k_o.t])
                tt(kb, "dve", r1[0:16, :], kr[:, 1, :], cst[0:16, :], ALU.mult, [kr.t, trk], [r1.t])
                tt(kb, "pool", r2[0:16, :], kr[:, 0, :], snt[0:16, :], ALU.mult, [kr.t, trk], [r2.t])
                tt(kb, "dve", k_o[:, 1, :], r1[0:16, :], r2[0:16, :], ALU.add, [r1.t, r2.t], [k_o.t])
                kb.dma(C.KRD[:, cols].rearrange("(two r) t -> r two t", two=2), k_o[:], reads=[k_o.t], writes=[C.KRD.t])
            kb.barrier()
            kb.flush()
        import os
        if os.environ.get("MLA_PART") == "A":
            return
        mlab = os.environ.get("MLA_B", "sc,asel,pv,fin").split(",")
        kb.dma(KrT[0:32, :], C.KRD[:], reads=[C.KRD.t], writes=[KrT.t])
        SC = [kb.psum(f"mlSC{i}", [128, 512], F32, es=es) for i in range(3)]
        OB = [kb.psum(f"mlO{i}", [128, 512], F32, es=es) for i in range(2)]
        TRb = kb.psum("mlTR", [128, 1024], BF16, es=es)
        PTs = [kb.sbuf(f"mPT{i}", [128, 384], BF16, es=es) for i in range(4)]
        QrTs = [kb.sbuf(f"QrT{i}", [128, 6, 128], BF16, es=es) for i in range(2)]
        for t_ in QrTs:
            memset(kb, "pool", t_[:], 0.0, [t_.t])
        ob = kb.sbuf("mob", [128, 6, 66], F32, es=es)
        rcp = kb.sbuf("mrc", [128, 6], F32, es=es)
        on = kb.sbuf("mon", [128, 6, 64], F32, es=es)
        onb = kb.sbuf("monb", [128, 384], BF16, es=es)
        obTs = [kb.sbuf(f"mobT{i}", [128, 3, 128], BF16, es=es) for i in range(2)]
        u = 0
        for i in range(NT):
            qc = slice(i * 128, (i + 1) * 128)
            QrT = QrTs[i % 2]
            for two in range(2):
                kb.dma(QrT[two * 16:(two + 1) * 16, :, :], C.QRD[two, :, :, qc].rearrange("h r t -> r h t"), reads=[C.QRD.t], writes=[QrT.t])
            O = OB[i % 2]
            first = True
            pend = None
            for grp in range(2):
                for j in range(i + 1):
                    kc = slice(j * 128, (j + 1) * 128)
                    bank = SC[u % 3]
                    PT = PTs[u % 4]
                    u += 1
                    if "sc" not in mlab:
                        continue
                    for g in range(3):
                        h = grp * 3 + g
                        c, hh = h // 2, h % 2
                        pr = slice(hh * 64, (hh + 1) * 64)
                        mm(kb, bank[:, g * 128:(g + 1) * 128], KnT[:, h, kc], QnT[:, c, qc], g == 0, [KnT.t, QnT.t], [bank.t])
                    mm(kb, bank[:, 0:384], KrT[:, kc], QrT[:, grp * 3:(grp + 1) * 3, :], False, [KrT.t, QrT.t], [bank.t])
                    if pend is not None:
                        PTp, jp, grpp = pend
                        for g in range(3):
                            h = grpp * 3 + g
                            mm(kb, O[:, h * 80:h * 80 + 66], PTp[:, g * 128:(g + 1) * 128], Vm[:, jp, h, :], first, [PTp.t, Vm.t], [O.t])
                            first = False
                        pend = None
                    act(kb, PT[:, 0:384], bank[:, 0:384], AF.Exp, [], [bank.t, PT.t], scale=sc)
                    if j == i and "asel" in mlab:
                        asel(kb, PT[:].rearrange("p (g q) -> p g q", q=128), PT[:].rearrange("p (g q) -> p g q", q=128),
                             [[0, 3], [1, 128]], ALU.is_ge, 0.0, 0, -1, [PT.t], [PT.t])
                    pend = (PT, j, grp)
            if pend is not None:
                PTp, jp, grpp = pend
                for g in range(3):
                    h = grpp * 3 + g
                    mm(kb, O[:, h * 80:h * 80 + 66], PTp[:, g * 128:(g + 1) * 128], Vm[:, jp, h, :], first, [PTp.t, Vm.t], [O.t])
                    first = False
                pend = None
            if "fin" not in mlab:
                continue
            tcopy(kb, "act", ob[:], O[:, 0:480].rearrange("p (h d) -> p h d", h=6)[:, :, 0:66], [], [O.t, ob.t])
            kb.op("dve", lambda e: e.reciprocal(out=rcp[:], in_=ob[:, :, 64]), [ob.t], [rcp.t])
            tt(kb, "dve", on[:], ob[:, :, 0:64], bcast(rcp[:], [(2, 64)]), ALU.mult, [ob.t, rcp.t], [on.t])
            rb = rstd_of(kb, C, on[:].rearrange("p h d -> p (h d)"), 384, [on.t], None)
            ts(kb, "dve", onb[:], on[:].rearrange("p h d -> p (h d)"), rb[:, 0:1], None, ALU.mult, None, [on.t, rb.t], [onb.t])
            obT = obTs[i % 2]
            for c in range(3):
                tr(kb, TRb[:, c * 128:(c + 1) * 128], onb[:, c * 128:(c + 1) * 128], C.ident[:], [onb.t, C.ident.t], [TRb.t])
            tcopy(kb, "act", obT[:], TRb[:, 0:384].rearrange("p (c t) -> p c t", c=3), [], [TRb.t, obT.t])
            kb.dma(C.MIXT[i, :, 3:6, :], obT[:], reads=[obT.t], writes=[C.MIXT.t])
        kb.barrier()
        kb.flush()


def ssm_phase(kb, C, l):
    P = C.P
    TB = 1024
    with contextlib.ExitStack() as es:
        make_stage(kb, C, es, 512)
        TRb = kb.psum("sTR", [128, 1024], BF16, es=es)
        BR = [kb.psum(f"sBR{i}", [128, 512], F32, es=es) for i in range(2)]
        BI = [kb.psum(f"sBI{i}", [128, 512], F32, es=es) for i in range(2)]
        Y = [kb.psum(f"sY{i}", [128, 512], F32, es=es) for i in range(2)]
        UTs = kb.sbuf("UTs", [128, 2, S], BF16, es=es)
        zT = kb.sbuf("zT", [128, 2, S], BF16, es=es)
        kb.dma(UTs[:], C.UT[:].rearrange("(c p) t -> p c t", p=128), reads=[C.UT.t], writes=[UTs.t])
        def pl(name):
            return P[name][l].rearrange("(q two) p -> two p q", two=2)
        prm = kb.sbuf("sprm", [128, 24, 8], F32, es=es)
        pt = prm.t
        (DT, ARE, AIM, LRE, TH, R, CO, SI, NR, DEN, T1, T2, CRE, CIM, FT, TI) = range(16)
        load_cols(kb, C, prm[:, ARE, :], pt, P["ssm_a_re"][l].rearrange("g p -> (g p)"), 8, Y[0])
        load_cols(kb, C, prm[:, AIM, :], pt, P["ssm_a_im"][l].rearrange("g p -> (g p)"), 8, Y[1])
        ldt = kb.sbuf("sldt", [128, 16], F32, es=es)
        kb.dma(ldt[:], bass.AP(P["ssm_log_dt"].h.tensor, l * 16, [[0, 128], [1, 16]]), writes=[ldt.t])
        for two in range(2):
            pr = slice(two * 64, (two + 1) * 64)
            tcopy(kb, "dve", prm[pr, DT, :], ldt[pr, :].rearrange("p (q two) -> p two q", two=2)[:, two, :], [ldt.t], [pt])
        act(kb, prm[:, DT, :], prm[:, DT, :], AF.Exp, [pt], [pt])
        tt(kb, "dve", prm[:, LRE, :], prm[:, ARE, :], prm[:, DT, :], ALU.mult, [pt], [pt])
        tt(kb, "dve", prm[:, TH, :], prm[:, AIM, :], prm[:, DT, :], ALU.mult, [pt], [pt])
        act(kb, prm[:, R, :], prm[:, LRE, :], AF.Exp, [pt], [pt])
        ts(kb, "dve", prm[:, FT, :], prm[:, TH, :], 1.0 / TWO_PI, None, ALU.mult, None, [pt], [pt])
        tcopy(kb, "dve", prm[:, T1, :], prm[:, FT, :], [pt], [pt])
        ti = kb.sbuf("sti", [128, 8], I32, es=es)
        sincos_from_turns(kb, prm[:, T1, :], prm[:, SI, :], prm[:, CO, :], ti[:], pt, [])
        tt(kb, "dve", prm[:, NR, :], prm[:, R, :], prm[:, CO, :], ALU.mult, [pt], [pt])
        ts(kb, "dve", prm[:, NR, :], prm[:, NR, :], -1.0, None, ALU.add, None, [pt], [pt])
        tt(kb, "dve", prm[:, T1, :], prm[:, R, :], prm[:, SI, :], ALU.mult, [pt], [pt])
        tt(kb, "dve", prm[:, DEN, :], prm[:, ARE, :], prm[:, ARE, :], ALU.mult, [pt], [pt])
        tt(kb, "dve", prm[:, T2, :], prm[:, AIM, :], prm[:, AIM, :], ALU.mult, [pt], [pt])
        tt(kb, "dve", prm[:, DEN, :], prm[:, DEN, :], prm[:, T2, :], ALU.add, [pt], [pt])
        kb.op("dve", lambda e: e.reciprocal(out=prm[:, DEN, :], in_=prm[:, DEN, :]), [pt], [pt])
        tt(kb, "dve", prm[:, CRE, :], prm[:, NR, :], prm[:, ARE, :], ALU.mult, [pt], [pt])
        tt(kb, "dve", prm[:, T2, :], prm[:, T1, :], prm[:, AIM, :], ALU.mult, [pt], [pt])
        tt(kb, "dve", prm[:, CRE, :], prm[:, CRE, :], prm[:, T2, :], ALU.add, [pt], [pt])
        tt(kb, "dve", prm[:, CRE, :], prm[:, CRE, :], prm[:, DEN, :], ALU.mult, [pt], [pt])
        tt(kb, "dve", prm[:, CIM, :], prm[:, T1, :], prm[:, ARE, :], ALU.mult, [pt], [pt])
        tt(kb, "dve", prm[:, T2, :], prm[:, NR, :], prm[:, AIM, :], ALU.mult, [pt], [pt])
        tt(kb, "dve", prm[:, CIM, :], prm[:, CIM, :], prm[:, T2, :], ALU.subtract, [pt], [pt])
        tt(kb, "dve", prm[:, CIM, :], prm[:, CIM, :], prm[:, DEN, :], ALU.mult, [pt], [pt])
        Braw = kb.sbuf("sBraw", [128, 2, 8, 16], F32, es=es)
        for ri, nm in enumerate(("ssm_b_re", "ssm_b_im")):
            for two in range(2):
                kb.dma(Braw[two * 64:(two + 1) * 64, ri, :, :], P[nm][l].rearrange("(q two) p c -> two p q c", two=2)[two], writes=[Braw.t])
        bbr = kb.sbuf("sbbr", [128, 8, 16], F32, es=es)
        bbi = kb.sbuf("sbbi", [128, 8, 16], F32, es=es)
        btmp = kb.sbuf("sbtmp", [128, 8, 16], F32, es=es)
        cre_b = bcast(prm[:, CRE, :], [(2, 16)])
        cim_b = bcast(prm[:, CIM, :], [(2, 16)])
        tt(kb, "dve", bbr[:], Braw[:, 0], cre_b, ALU.mult, [Braw.t, pt], [bbr.t])
        tt(kb, "dve", btmp[:], Braw[:, 1], cim_b, ALU.mult, [Braw.t, pt], [btmp.t])
        tt(kb, "dve", bbr[:], bbr[:], btmp[:], ALU.subtract, [bbr.t, btmp.t], [bbr.t])
        tt(kb, "dve", bbi[:], Braw[:, 1], cre_b, ALU.mult, [Braw.t, pt], [bbi.t])
        tt(kb, "dve", btmp[:], Braw[:, 0], cim_b, ALU.mult, [Braw.t, pt], [btmp.t])
        tt(kb, "dve", bbi[:], bbi[:], btmp[:], ALU.add, [bbi.t, btmp.t], [bbi.t])
        Zb = kb.sbuf("sZb", [128, 128], BF16, es=es)
        LB = kb.sbuf("sLB", [128, 2, 8, 128], BF16, es=es)
        LC = kb.sbuf("sLC", [128, 4, 8, 128], BF16, es=es)
        for ri, bb in enumerate((bbr, bbi)):
            for q in range(8):
                memset(kb, "pool", Zb[:], 0.0, [Zb.t])
                c0 = 32 * (q % 4)
                tcopy(kb, "dve", Zb[0:64, c0:c0 + 16], bb[0:64, q, :], [bb.t], [Zb.t])
                tcopy(kb, "dve", Zb[64:128, c0 + 16:c0 + 32], bb[64:128, q, :], [bb.t], [Zb.t])
                tr(kb, TRb[:, 0:128], Zb[:], C.ident[:], [Zb.t, C.ident.t], [TRb.t])
                tcopy(kb, "act", LB[:, ri, q, :], TRb[:, 0:128], [], [TRb.t, LB.t])
        memset(kb, "pool", LC[:], 0.0, [LC.t])
        Xc = kb.sbuf("sXc", [128, 128], F32, es=es)
        Xcb = kb.sbuf("sXcb", [128, 128], BF16, es=es)
        XT = kb.sbuf("sXT", [128, 128], BF16, es=es)
        XTn = kb.sbuf("sXTn", [128, 128], BF16, es=es)
        for ri, nm in enumerate(("ssm_c_re", "ssm_c_im")):
            for q in range(8):
                kb.dma(Xc[q * 16:(q + 1) * 16, :].rearrange("p (two s) -> p two s", two=2),
                       P[nm][l, 2 * q:2 * q + 2].rearrange("two c p -> c two p"), writes=[Xc.t])
            tcopy(kb, "dve", Xcb[:], Xc[:], [Xc.t], [Xcb.t])
            tr(kb, TRb[:, 128:256], Xcb[:], C.ident[:], [Xcb.t, C.ident.t], [TRb.t])
            tcopy(kb, "act", XT[:], TRb[:, 128:256], [], [TRb.t, XT.t])
            ts(kb, "dve", XTn[:], XT[:], -1.0, None, ALU.mult, None, [XT.t], [XTn.t])
            for q in range(8):
                c0 = 32 * (q % 4)
                for two in range(2):
                    pr = slice(two * 64, (two + 1) * 64)
                    dst = slice(c0 + 16 * two, c0 + 16 * two + 16)
                    if ri == 0:
                        tcopy(kb, "dve", LC[pr, 0, q, dst], XT[pr, q * 16:(q + 1) * 16], [XT.t], [LC.t])
                        tcopy(kb, "pool", LC[pr, 1, q, dst], XTn[pr, q * 16:(q + 1) * 16], [XTn.t], [LC.t])
                    else:
                        tcopy(kb, "dve", LC[pr, 2, q, dst], XTn[pr, q * 16:(q + 1) * 16], [XTn.t], [LC.t])
        dcol = kb.sbuf("sdcol", [128, 4], F32, es=es)
        load_cols(kb, C, dcol[:, 0:2], dcol.t, P["ssm_d"][l].rearrange("g c -> (g c)"), 2, Y[0])
        load_cols(kb, C, dcol[:, 2:4], dcol.t, P["ssm_b_glu"][l], 2, Y[1])
        Wg = kb.sbuf("sWg", [128, 2, 256], BF16, es=es)
        for kc in range(2):
            st = stage_load(kb, C, P["ssm_w_glu"][l, kc * 128:(kc + 1) * 128, :], 128, 256)
            cast_scaled(kb, C, Wg[:, kc, :], st[:, 0:256], None, [st.t], [Wg.t])
        tidx = kb.sbuf("stidx", [128, TB], F32, es=es)
        kb.op("pool", lambda e: e.iota(tidx[:], [[1, TB]], base=0, channel_multiplier=0, allow_small_or_imprecise_dtypes=True), [], [tidx.t])
        rdecs = [kb.sbuf(f"srdec{i}", [128, TB], F32, es=es) for i in range(2)]
        basef = kb.sbuf("sbasef", [128, S // TB, 8], F32, es=es)
        basei = kb.sbuf("sbasei", [128, S // TB, 8], I32, es=es)
        for tbk in range(S // TB):
            ts(kb, "dve", basef[:, tbk, :], prm[:, FT, :], float(tbk * TB), None, ALU.mult, None, [pt], [basef.t])
        tcopy(kb, "dve", basei[:], basef[:], [basef.t], [basei.t])
        tt(kb, "dve", basef[:], basef[:], basei[:], ALU.subtract, [basef.t, basei.t], [basef.t])
        xs = [kb.sbuf(f"sx{i}", [128, TB], F32, es=es) for i in range(2)]
        xi = kb.sbuf("sxi", [128, TB], I32, es=es)
        sns = [kb.sbuf(f"ssn{i}", [128, TB], F32, es=es) for i in range(2)]
        css = [kb.sbuf(f"scs{i}", [128, TB], F32, es=es) for i in range(2)]
        bre = kb.sbuf("sbre", [128, TB], F32, es=es)
        bim = kb.sbuf("sbim", [128, TB], F32, es=es)
        ta = kb.sbuf("sta", [128, TB], F32, es=es)
        tb_ = kb.sbuf("stb", [128, TB], F32, es=es)
        tc_ = kb.sbuf("stc", [128, TB], F32, es=es)
        td = kb.sbuf("std", [128, TB], F32, es=es)
        gre = kb.sbuf("sgre", [128, TB], F32, es=es)
        gim = kb.sbuf("sgim", [128, TB], F32, es=es)
        G = [kb.sbuf(f"sG{i}", [128, 4, TB], BF16, es=es) for i in range(2)]
        state = kb.sbuf("sstate", [128, 8, 2], F32, es=es)
        memset(kb, "pool", state[:], 0.0, [state.t])
        yb = kb.sbuf("syb", [128, TB], F32, es=es)
        y1 = kb.sbuf("sy1", [128, TB], F32, es=es)
        y2 = kb.sbuf("sy2", [128, TB], F32, es=es)
        it = 0
        for ch in range(2):
            for tbk in range(S // TB):
                t0 = tbk * TB
                for qq in range(4):
                    q = ch * 4 + qq
                    x = xs[it % 2]; snt = sns[it % 2]; cst = css[it % 2]; rdec = rdecs[it % 2]
                    it += 1
                    ts(kb, "pool", rdec[:], tidx[:], 0.0, prm[:, R, q:q + 1], ALU.mult, ALU.add, [tidx.t, pt], [rdec.t])
                    ts(kb, "dve", x[:], tidx[:], prm[:, FT, q:q + 1], basef[:, tbk, q:q + 1], ALU.mult, ALU.add, [tidx.t, pt, basef.t], [x.t])
                    sincos_from_turns(kb, x[:], snt[:], cst[:], xi[:], x.t, [])
                    trk = x.t
                    for hb_ in range(2):
                        tc0 = t0 + hb_ * 512
                        mm(kb, BR[hb_][:, :], LB[:, 0, q, :], UTs[:, ch, tc0:tc0 + 512], True, [LB.t, UTs.t], [BR[hb_].t])
                        mm(kb, BI[hb_][:, :], LB[:, 1, q, :], UTs[:, ch, tc0:tc0 + 512], True, [LB.t, UTs.t], [BI[hb_].t])
                        tcopy(kb, "act", bre[:, hb_ * 512:(hb_ + 1) * 512], BR[hb_][:, :], [], [BR[hb_].t, bre.t])
                        tcopy(kb, "act", bim[:, hb_ * 512:(hb_ + 1) * 512], BI[hb_][:, :], [], [BI[hb_].t, bim.t])
                    tt(kb, "dve", ta[:], bre[:], cst[:], ALU.mult, [bre.t, trk], [ta.t])
                    tt(kb, "pool", tb_[:], bim[:], snt[:], ALU.mult, [bim.t, trk], [tb_.t])
                    tt(kb, "dve", ta[:], ta[:], tb_[:], ALU.add, [ta.t, tb_.t], [ta.t])
                    tt(kb, "pool", tc_[:], bim[:], cst[:], ALU.mult, [bim.t, trk], [tc_.t])
                    tt(kb, "pool", td[:], bre[:], snt[:], ALU.mult, [bre.t, trk], [td.t])
                    tt(kb, "dve", tc_[:], tc_[:], td[:], ALU.subtract, [tc_.t, td.t], [tc_.t])
                    kb.op("dve", lambda e, q=q, rdec=rdec: e.tensor_tensor_scan(out=gre[:], data0=rdec[:], data1=ta[:], initial=state[:, q, 0:1],
                                                                     op0=ALU.mult, op1=ALU.add), [rdec.t, ta.t, state.t], [gre.t])
                    kb.op("dve", lambda e, q=q, rdec=rdec: e.tensor_tensor_scan(out=gim[:], data0=rdec[:], data1=tc_[:], initial=state[:, q, 1:2],
                                                                     op0=ALU.mult, op1=ALU.add), [rdec.t, tc_.t, state.t], [gim.t])
                    tcopy(kb, "dve", state[:, q, 0:1], gre[:, TB - 1:TB], [gre.t], [state.t])
                    tcopy(kb, "dve", state[:, q, 1:2], gim[:, TB - 1:TB], [gim.t], [state.t])
                    Gq = G[qq % 2]
                    tt(kb, "dve", Gq[:, 0, :], gre[:], cst[:], ALU.mult, [gre.t, trk], [Gq.t])
                    tt(kb, "pool", Gq[:, 1, :], gim[:], snt[:], ALU.mult, [gim.t, trk], [Gq.t])
                    tt(kb, "pool", Gq[:, 2, :], gre[:], snt[:], ALU.mult, [gre.t, trk], [Gq.t])
                    tt(kb, "dve", Gq[:, 3, :], gim[:], cst[:], ALU.mult, [gim.t, trk], [Gq.t])
                    for hb_ in range(2):
                        cs_ = slice(hb_ * 512, (hb_ + 1) * 512)
                        mm(kb, Y[hb_][:, :], LC[:, 0, q, :], Gq[:, 0, cs_], qq == 0, [LC.t, Gq.t], [Y[hb_].t])
                        mm(kb, Y[hb_][:, :], LC[:, 1, q, :], Gq[:, 1, cs_], False, [LC.t, Gq.t], [Y[hb_].t])
                        mm(kb, Y[hb_][:, :], LC[:, 2, q, :], Gq[:, 2, cs_], False, [LC.t, Gq.t], [Y[hb_].t])
                        mm(kb, Y[hb_][:, :], LC[:, 2, q, :], Gq[:, 3, cs_], False, [LC.t, Gq.t], [Y[hb_].t])
                for hb_ in range(2):
                    tc0 = t0 + hb_ * 512
                    stt(kb, yb[:, hb_ * 512:(hb_ + 1) * 512], UTs[:, ch, tc0:tc0 + 512], dcol[:, ch:ch + 1], Y[hb_][:, :], ALU.mult, ALU.add,
                        [UTs.t, dcol.t], [Y[hb_].t, yb.t])
                gelu_tanh(kb, zT[:, ch, t0:t0 + TB], yb[:], y1[:], y2[:], [yb.t], [zT.t], y1.t)
        ocs = [kb.sbuf(f"socs{i}", [128, 2, 512], BF16, es=es) for i in range(2)]
        sg = kb.sbuf("ssg", [128, 512], F32, es=es)
        gi = 0
        for tg in range(8):
            cols = slice(tg * 512, (tg + 1) * 512)
            for oc_ in range(2):
                bank = Y[gi % 2]
                o_st = ocs[gi % 2]
                gi += 1
                for kc in range(2):
                    mm(kb, bank[:, :], Wg[:, kc, oc_ * 128:(oc_ + 1) * 128], zT[:, kc, cols], kc == 0, [Wg.t, zT.t], [bank.t])
                act(kb, sg[:], bank[:, :], AF.Sigmoid, [dcol.t], [bank.t, sg.t], bias=dcol[:, 2 + oc_:3 + oc_])
                tt(kb, "dve", o_st[:, 0, :], zT[:, oc_, cols], sg[:], ALU.mult, [zT.t, sg.t], [o_st.t])
                tt(kb, "pool", o_st[:, 1, :], o_st[:, 0, :], o_st[:, 0, :], ALU.mult, [o_st.t], [o_st.t])
                for sq_ in range(2):
                    kb.dma(C.MIXT[tg * 4:(tg + 1) * 4, :, 6 + 2 * sq_ + oc_, :].rearrange("n p t -> p n t"),
                           o_st[:, sq_, :].rearrange("p (n t) -> p n t", n=4), reads=[o_st.t], writes=[C.MIXT.t])
        kb.barrier()
        kb.flush()


def outproj_phase(kb, C, l):
    P = C.P
    with contextlib.ExitStack() as es:
        Wo = kb.sbuf("Wo", [128, 8, D], BF16, es=es)
        make_stage(kb, C, es, 1024)
        gc = kb.sbuf("ogc", [128, 8], F32, es=es)
        AB = [kb.psum(f"oAB{i}", [128, 512], F32, es=es) for i in range(2)]
        CC = [kb.psum(f"oCC{i}", [128, 512], F32, es=es) for i in range(2)]
        SQ = kb.psum("oSQ", [128, 512], F32, es=es)
        load_cols(kb, C, gc[:, 0:3], gc.t, P["out_norm_nsa"][l], 3, AB[0])
        load_cols(kb, C, gc[:, 3:6], gc.t, P["out_norm_mla"][l], 3, AB[1])
        load_cols(kb, C, gc[:, 6:8], gc.t, P["out_norm_ssm"][l], 2, CC[0])
        for kc in range(8):
            st = stage_load(kb, C, P["w_out"][l, kc * 128:(kc + 1) * 128, :], 128, D)
            cast_scaled(kb, C, Wo[:, kc, :], st[:, 0:D], gc[:, kc:kc + 1], [st.t, gc.t], [Wo.t])
        mts = [kb.sbuf(f"omt{i}", [128, 10, 128], BF16, es=es) for i in range(2)]
        xbs = [kb.sbuf(f"oxb{i}", [128, D], F32, es=es) for i in range(2)]
        xo = [kb.sbuf(f"oxo{i}", [128, D], F32, es=es) for i in range(2)]
        rsc = kb.sbuf("orsc", [128, 4], F32, es=es)
        rscw = kb.sbuf("orscw", [128, 128], F32, es=es)
        xin = C.x_d if l == 0 else C.XR
        for i in range(NT):
            qc = slice(i * 128, (i + 1) * 128)
            mt = mts[i % 2]
            xt = xbs[i % 2]
            xn = xo[i % 2]
            kb.dma(mt[:], C.MIXT[i], reads=[C.MIXT.t], writes=[mt.t])
            kb.dma(xt[:], xin[qc, :], reads=[xin.t], writes=[xt.t])
            for hb_ in range(2):
                cs_ = slice(hb_ * 512, (hb_ + 1) * 512)
                for c in range(6):
                    mm(kb, AB[hb_][:, :], mt[:, c, :], Wo[:, c, cs_], c == 0, [mt.t, Wo.t], [AB[hb_].t])
                for c in range(6, 8):
                    mm(kb, CC[hb_][:, :], mt[:, c, :], Wo[:, c, cs_], c == 6, [mt.t, Wo.t], [CC[hb_].t])
            for c in range(2):
                mm(kb, SQ[:, 0:128], mt[:, 8 + c, :], C.ones[:, 0:128], c == 0, [mt.t, C.ones.t], [SQ.t])
            ts(kb, "dve", rscw[:], SQ[:, 0:128], 1.0 / 256.0, EPS, ALU.mult, ALU.add, [], [SQ.t, rscw.t])
            tcopy(kb, "dve", rsc[:, 0:1], rscw[:, 0:1], [rscw.t], [rsc.t])
            act(kb, rsc[:, 1:2], rsc[:, 0:1], AF.Sqrt, [rsc.t], [rsc.t])
            kb.op("dve", lambda e: e.reciprocal(out=rsc[:, 2:3], in_=rsc[:, 1:2]), [rsc.t], [rsc.t])
            for hb_ in range(2):
                cs_ = slice(hb_ * 512, (hb_ + 1) * 512)
                tt(kb, "dve", xn[:, cs_], AB[hb_][:, :], xt[:, cs_], ALU.add, [xt.t], [AB[hb_].t, xn.t])
                stt(kb, xn[:, cs_], CC[hb_][:, :], rsc[:, 2:3], xn[:, cs_], ALU.mult, ALU.add, [rsc.t], [CC[hb_].t, xn.t])
            kb.dma(C.XR[qc, :], xn[:], reads=[xn.t], writes=[C.XR.t])
        kb.barrier()
        kb.flush()


def ffn_phase(kb, C, l, last):
    P = C.P
    NF = DFF // 128
    TG = 256
    with contextlib.ExitStack() as es:
        make_stage(kb, C, es, 1024)
        Wup = kb.sbuf("Wup", [128, 8, 2 * DFF], BF16, es=es)
        Wdn = kb.sbuf("Wdn", [128, NF, D], BF16, es=es)
        gc = kb.sbuf("fgc", [128, 8], F32, es=es)
        TR = kb.psum("fTR", [128, 1024], BF16, es=es)
        GB = [kb.psum(f"fG{i}", [128, 512], F32, es=es) for i in range(1)]
        VB = [kb.psum(f"fV{i}", [128, 512], F32, es=es) for i in range(1)]
        DB = [[kb.psum(f"fD{t}{h}", [128, 512], F32, es=es) for h in range(2)] for t in range(2)]
        load_cols(kb, C, gc[:], gc.t, P["ffn_norm"][l], 8, DB[0][0])
        cw = kb.sbuf("fcw", [128, 4, NF], F32, es=es)
        for k in range(3):
            load_cols(kb, C, cw[:, k, :], cw.t, P["ffn_conv_w"][l, k], NF, DB[0][1] if k % 2 else DB[1][0])
        load_cols(kb, C, cw[:, 3, :], cw.t, P["ffn_conv_b"][l], NF, DB[1][1])
        for kc in range(8):
            for part in range(6):
                c0 = part * 1024
                w = min(1024, 2 * DFF - c0)
                st = stage_load(kb, C, P["ffn_w_up"][l, kc * 128:(kc + 1) * 128, c0:c0 + w], 128, w)
                cast_scaled(kb, C, Wup[:, kc, c0:c0 + w], st[:, 0:w], gc[:, kc:kc + 1], [st.t, gc.t], [Wup.t])
        for fc in range(NF):
            st = stage_load(kb, C, P["ffn_w_down"][l, fc * 128:(fc + 1) * 128, :], 128, D)
            cast_scaled(kb, C, Wdn[:, fc, :], st[:, 0:D], None, [st.t], [Wdn.t])
        if last:
            fg = kb.sbuf("ffg", [128, D], F32, es=es)
            kb.dma(fg[:], bass.AP(P["final_norm"].h.tensor, 0, [[0, 128], [1, D]]), writes=[fg.t])
        x1s = [kb.sbuf(f"fx1{i}", [128, 2, D], F32, es=es) for i in range(2)]
        xns = [kb.sbuf(f"fxn{i}", [128, D], BF16, es=es) for i in range(2)]
        hT = kb.sbuf("fhT", [128, 8, TG], BF16, es=es)
        aTs = [kb.sbuf(f"faT{i}", [128, TG], BF16, es=es) for i in range(2)]
        halo = kb.sbuf("fhalo", [128, NF, 2], F32, es=es)
        memset(kb, "pool", halo[:], 0.0, [halo.t])
        Gs = [kb.sbuf(f"fGs{i}", [128, TG + 2], F32, es=es) for i in range(2)]
        cv = [kb.sbuf(f"fcv{i}", [128, TG], F32, es=es) for i in range(2)]
        sl = [kb.sbuf(f"fsl{i}", [128, TG], F32, es=es) for i in range(2)]
        it = 0
        for tg in range(S // TG):
            x1 = x1s[tg % 2]
            for tl in range(2):
                tt_ = tg * 2 + tl
                kb.dma(x1[:, tl, :], C.XR[tt_ * 128:(tt_ + 1) * 128, :], reads=[C.XR.t], writes=[x1.t])
                rs = rstd_of(kb, C, x1[:, tl, :], D, [x1.t], None)
                xn = xns[tt_ % 2]
                ts(kb, "dve", xn[:], x1[:, tl, :], rs[:, 0:1], None, ALU.mult, None, [x1.t, rs.t], [xn.t])
                for c in range(8):
                    tr(kb, TR[:, c * 128:(c + 1) * 128], xn[:, c * 128:(c + 1) * 128], C.ident[:], [xn.t, C.ident.t], [TR.t])
                tcopy(kb, "act", hT[:, :, tl * 128:(tl + 1) * 128], TR[:].rearrange("p (c t) -> p c t", c=8), [], [TR.t, hT.t])
            for fc in range(NF):
                gb_ = GB[0]; vb = VB[0]; G_ = Gs[it % 2]; c_ = cv[it % 2]; s_ = sl[it % 2]; aT = aTs[it % 2]
                it += 1
                for kc in range(8):
                    mm(kb, gb_[:, 0:TG], Wup[:, kc, fc * 128:(fc + 1) * 128], hT[:, kc, :], kc == 0, [Wup.t, hT.t], [gb_.t])
                for kc in range(8):
                    mm(kb, vb[:, 0:TG], Wup[:, kc, DFF + fc * 128:DFF + (fc + 1) * 128], hT[:, kc, :], kc == 0, [Wup.t, hT.t], [vb.t])
                tcopy(kb, "pool", G_[:, 0:2], halo[:, fc, :], [halo.t], [G_.t])
                tcopy(kb, "act", G_[:, 2:TG + 2], gb_[:, 0:TG], [], [gb_.t, G_.t])
                tcopy(kb, "pool", halo[:, fc, :], G_[:, TG:TG + 2], [G_.t], [halo.t])
                ts(kb, "dve", c_[:], G_[:, 2:TG + 2], cw[:, 2, fc:fc + 1], cw[:, 3, fc:fc + 1], ALU.mult, ALU.add, [G_.t, cw.t], [c_.t])
                stt(kb, c_[:], G_[:, 1:TG + 1], cw[:, 1, fc:fc + 1], c_[:], ALU.mult, ALU.add, [G_.t, cw.t], [c_.t])
                stt(kb, c_[:], G_[:, 0:TG], cw[:, 0, fc:fc + 1], c_[:], ALU.mult, ALU.add, [G_.t, cw.t], [c_.t])
                act(kb, s_[:], c_[:], AF.Silu, [c_.t], [s_.t])
                tt(kb, "dve", aT[:], vb[:, 0:TG], s_[:], ALU.mult, [s_.t], [vb.t, aT.t])
                for tl in range(2):
                    for hb_ in range(2):
                        mm(kb, DB[tl][hb_][:, :], aT[:, tl * 128:(tl + 1) * 128], Wdn[:, fc, hb_ * 512:(hb_ + 1) * 512], fc == 0,
                           [aT.t, Wdn.t], [DB[tl][hb_].t])
            for tl in range(2):
                tt_ = tg * 2 + tl
                for hb_ in range(2):
                    cs_ = slice(hb_ * 512, (hb_ + 1) * 512)
                    tt(kb, "dve", x1[:, tl, cs_], DB[tl][hb_][:, :], x1[:, tl, cs_], ALU.add, [x1.t], [DB[tl][hb_].t, x1.t])
                if last:
                    rs = rstd_of(kb, C, x1[:, tl, :], D, [x1.t], None)
                    stt(kb, x1[:, tl, :], x1[:, tl, :], rs[:, 0:1], fg[:], ALU.mult, ALU.mult, [x1.t, rs.t, fg.t], [x1.t])
                    kb.dma(C.out_d[tt_ * 128:(tt_ + 1) * 128, :], x1[:, tl, :], reads=[x1.t], writes=[C.out_d.t])
                else:
                    kb.dma(C.XR[tt_ * 128:(tt_ + 1) * 128, :], x1[:, tl, :], reads=[x1.t], writes=[C.XR.t])
        kb.barrier()
        kb.flush()


PARAM_SHAPES = {
    "attn_norm": (4, 1024), "w_in": (4, 1024, 1970), "nsa_pe": (4, 32, 64),
    "nsa_ck_w1": (4, 32, 64, 128), "nsa_ck_b1": (4, 128), "nsa_ck_w2": (4, 128, 64), "nsa_ck_b2": (4, 64),
    "nsa_cv_w1": (4, 32, 64, 128), "nsa_cv_b1": (4, 128), "nsa_cv_w2": (4, 128, 64), "nsa_cv_b2": (4, 64),
    "nsa_gate_b": (4, 18), "mla_q_norm": (4, 384), "mla_kv_norm": (4, 128), "mla_w_uq": (4, 384, 576),
    "mla_w_uk": (4, 128, 384), "mla_w_uv": (4, 128, 384), "ssm_log_dt": (4, 16), "ssm_a_re": (4, 16, 64),
    "ssm_a_im": (4, 16, 64), "ssm_b_re": (4, 16, 64, 16), "ssm_b_im": (4, 16, 64, 16), "ssm_c_re": (4, 16, 16, 64),
    "ssm_c_im": (4, 16, 16, 64), "ssm_d": (4, 16, 16), "ssm_w_glu": (4, 256, 256), "ssm_b_glu": (4, 256),
    "out_norm_nsa": (4, 384), "out_norm_mla": (4, 384), "out_norm_ssm": (4, 256), "w_out": (4, 1024, 1024),
    "ffn_norm": (4, 1024), "ffn_w_up": (4, 1024, 5632), "ffn_conv_w": (4, 3, 2816), "ffn_conv_b": (4, 2816),
    "ffn_w_down": (4, 2816, 1024), "final_norm": (1024,),
}


def build_program(depth=DEPTH, phases=("p1", "nsa", "mla", "ssm", "out", "ffn"), dbg=()):
    nc = bass.Bass("TRN2", target_bir_lowering=False)
    kb = KB(nc)
    C = Ctx()
    C.x_d = kb.dram("x", [S, D], F32, kind="ExternalInput")
    C.pos_d = kb.dram("positions", [1, S], I32, kind="ExternalInput")
    C.invf_d = kb.dram("invf", [96, 1], F32, kind="ExternalInput")
    C.P = {k: kb.dram(k, list(v), F32, kind="ExternalInput") for k, v in PARAM_SHAPES.items()}
    C.out_d = kb.dram("out", [S, D], F32, kind="ExternalOutput")
    C.XR = kb.dram("XR", [S, D], F32)
    C.UT = kb.dram("UT", [256, S], BF16)
    C.LATT = kb.dram("LATT", [512, S], BF16)
    C.KR12 = kb.dram("KR12", [32, S], F32)
    C.MIXT = kb.dram("MIXT", [NT, 128, 10, 128], BF16)
    C.QRD = kb.dram("QRD", [2, 6, 16, S], BF16)
    C.KRD = kb.dram("KRD", [32, S], BF16)
    C.dbg = {}
    setup_consts_pre(kb, C)
    for l in range(depth):
        with contextlib.ExitStack() as esL:
            LS = Ctx()
            LS.QaT = kb.sbuf("QaT", [128, 3, S], BF16, es=esL)
            LS.KcT = kb.sbuf("KcT", [128, S], BF16, es=esL)
            LS.VcT = kb.sbuf("VcT", [128, S], BF16, es=esL)
            LS.KsT = kb.sbuf("KsT", [128, 2, S], BF16, es=esL)
            LS.KwT = kb.sbuf("KwT", [128, 2, S], BF16, es=esL)
            memset(kb, "pool", LS.KsT[:], 0.0, [LS.KsT.t])
            memset(kb, "pool", LS.KwT[:], 0.0, [LS.KwT.t])
            LS.Vs = kb.sbuf("Vs", [128, NT, 2, 66], BF16, es=esL)
            LS.Vw = kb.sbuf("Vw", [128, NT, 2, 66], BF16, es=esL)
            LS.Gt = kb.sbuf("Gt", [128, NT, 18], F32, es=esL)
            memset(kb, "pool", LS.Vs[:], 1.0, [LS.Vs.t])
            memset(kb, "pool", LS.Vw[:], 1.0, [LS.Vw.t])
            if "p1" in phases:
                phase1(kb, C, l, LS)
            if "nsa" in phases:
                nsa_attention(kb, C, l, LS)
            kb.barrier()
            kb.flush()
        if "mla" in phases:
            mla_phase(kb, C, l)
        if "ssm" in phases:
            ssm_phase(kb, C, l)
        if "out" in phases:
            outproj_phase(kb, C, l)
        if "ffn" in phases:
            ffn_phase(kb, C, l, last=(l == depth - 1))
    for name in dbg:
        src = getattr(C, name)
        o = kb.dram("dbg_" + name, list(src.h.shape), src.h.dtype, kind="ExternalOutput")
        kb.dma(o[:], src[:], reads=[src.t], writes=[o.t])
    kb.flush(final=True)
    kb.close()
    return nc


def setup_consts_pre(kb, C):
    hp = kb.sbuf("halfpi", [128, 1], F32)
    memset(kb, "pool", hp[:], math.pi / 2.0, [hp.t])
    C_HALF_PI[0] = hp
    setup_consts(kb, C)
    kb.barrier()
    kb.flush()


_CACHE = {}


def host_consts():
    half = 16
    inv_freq = (10000.0 ** (-np.arange(half, dtype=np.float32) / half)).astype(np.float32)
    return {"invf": np.tile(inv_freq, 6).reshape(96, 1).astype(np.float32)}


def kernel(**inputs):
    if "nc" not in _CACHE:
        _CACHE["nc"] = build_program()
    nc = _CACHE["nc"]
    x = np.ascontiguousarray(inputs["x"], dtype=np.float32)
    pos = np.ascontiguousarray(inputs["positions"], dtype=np.int32)
    shared = {k: np.ascontiguousarray(inputs[k], dtype=np.float32) for k in PARAM_SHAPES}
    shared.update(host_consts())
    in_maps = []
    for b in range(8):
        m = dict(shared)
        m["x"] = x[b]
        m["positions"] = pos[b:b + 1]
        in_maps.append(m)
    res = run_bass_kernel_spmd(nc, in_maps, core_ids=list(range(8)))
    return np.stack([np.asarray(r["out"], dtype=np.float32) for r in res.results], axis=0)
```
